# Optimizing a Trainium2 kernel written in Bass

```python
import math
import jax
import jax.numpy as jnp
from jax import lax
import numpy as np

D_MODEL = 1024
BATCH = 8
SEQ = 4096
DEPTH = 1

ATT_HEAD_DIM = 64
ATT_HEADS = 8
ATT_GROUPS = ((128, 1), (512, 4), (2048, 16))
N_ATT_GROUPS = 3
ATT_QK_WIDTH = N_ATT_GROUPS * ATT_HEADS * ATT_HEAD_DIM
ATT_OUT_WIDTH = ATT_HEADS * ATT_HEAD_DIM
ATT_BLOCK = 128
NEG_INF = -1e30

HG_HEADS = 8
HG_EXPAND = 128
HG_WIDTH = HG_HEADS * HG_EXPAND
HG_HEAD_V = HG_WIDTH // HG_HEADS
HG_CHUNK = 32
RMS_EPS = 1e-6

SPLIT_SIZES = (ATT_QK_WIDTH,) * 3 + (HG_WIDTH,) * 4 + (D_MODEL,) * 2
N_IN = 3 * ATT_QK_WIDTH + 4 * HG_WIDTH + 2 * D_MODEL

N_EXPERTS = 64
TOP_K = 8
N_EXPERT_GROUPS = 8
TOPK_GROUPS = 4
EXPERT_FF = 256
SHARED_FF = 256
ROUTED_SCALE = 2.5
MOE_BLOCK = 512

PLE_DIM = 256
LN_EPS = 1e-5
DEEPNORM_ALPHA = (2.0 * DEPTH) ** 0.25
DEEPNORM_BETA = (8.0 * DEPTH) ** -0.25

kernel_name = 'hybrid_dilated_attn_hgrn2_moe_block'


def layer_norm(x, w, b):
    xf = x.astype(jnp.float32)
    mu = xf.mean(-1, keepdims=True)
    var = jnp.mean(jnp.square(xf - mu), -1, keepdims=True)
    return ((xf - mu) * lax.rsqrt(var + LN_EPS) * w.astype(jnp.float32) + b.astype(jnp.float32)).astype(x.dtype)


def alibi_slopes():
    return jnp.asarray(np.array([2.0 ** (-8.0 * (h + 1) / ATT_HEADS) for h in range(ATT_HEADS)], np.float32))


def dilated_window_group(q, k, v, window, dilation):
    B, S, H, Dh = q.shape
    w_sub = window // dilation
    L = S // dilation
    nb = -(-L // ATT_BLOCK)
    pad = nb * ATT_BLOCK - L

    def to_blocks(t):
        t = t.astype(jnp.float32).reshape(B, L, dilation, H, Dh)
        t = jnp.pad(t, ((0, 0), (0, pad), (0, 0), (0, 0), (0, 0)))
        return t.reshape(B, nb, ATT_BLOCK, dilation, H, Dh)

    def with_previous_block(t):
        prev = jnp.pad(t[:, :-1], ((0, 0), (1, 0), (0, 0), (0, 0), (0, 0), (0, 0)))
        return jnp.concatenate([prev, t], axis=2)

    qb = to_blocks(q) * (Dh ** -0.5)
    kc = with_previous_block(to_blocks(k))
    vc = with_previous_block(to_blocks(v))

    qi = jnp.arange(ATT_BLOCK)[:, None]
    ki = jnp.arange(2 * ATT_BLOCK)[None, :]
    steps = qi + ATT_BLOCK - ki
    key_idx = jnp.arange(nb)[:, None] * ATT_BLOCK - ATT_BLOCK + jnp.arange(2 * ATT_BLOCK)[None, :]
    valid = ((steps >= 0) & (steps <= w_sub))[None] & (key_idx >= 0)[:, None, :]
    bias = -alibi_slopes()[:, None, None] * (steps * dilation).astype(jnp.float32)[None]

    s = jnp.einsum('bnqrhd,bnkrhd->bnrhqk', qb, kc) + bias[None, None, None]
    s = jnp.where(valid[None, :, None, None], s, NEG_INF)
    m = s.max(-1)
    pexp = jnp.exp(s - m[..., None])
    den = pexp.sum(-1)
    num = jnp.einsum('bnrhqk,bnkrhd->bnqrhd', pexp, vc)

    def back(t):
        t = t.reshape((B, nb * ATT_BLOCK, dilation) + t.shape[4:])[:, :L]
        return t.reshape((B, S) + t.shape[3:])

    return back(num), back(m.transpose(0, 1, 4, 2, 3)), back(den.transpose(0, 1, 4, 2, 3))


def dilated_attention(q, k, v):
    parts = [dilated_window_group(q[:, :, g], k[:, :, g], v[:, :, g], win, dil)
             for g, (win, dil) in enumerate(ATT_GROUPS)]
    m_all = jnp.stack([pt[1] for pt in parts])
    scale = jnp.exp(m_all - m_all.max(0))
    num = sum(scale[g][..., None] * parts[g][0] for g in range(N_ATT_GROUPS))
    den = sum(scale[g] * parts[g][2] for g in range(N_ATT_GROUPS))
    return num / den[..., None]


def hgrn2_recurrence(q, f_logit, i, lb):
    B, S, _ = q.shape
    n_chunks = S // HG_CHUNK
    forget = lb + (1.0 - lb) * jax.nn.sigmoid(f_logit.astype(jnp.float32))
    log_f = jnp.log(forget)
    key = 1.0 - forget

    def chunked(t):
        t = t.astype(jnp.float32).reshape(B, n_chunks, HG_CHUNK, HG_HEADS, -1)
        return t.transpose(1, 0, 3, 2, 4)

    causal = jnp.tril(jnp.ones((HG_CHUNK, HG_CHUNK), dtype=bool))

    def step(state, inp):
        qc, kc, vc, gc = inp
        b = jnp.cumsum(gc, axis=2)
        b_last = b[:, :, -1:]
        q_dec = qc * jnp.exp(b)
        k_inv = kc * jnp.exp(-b)
        k_end = kc * jnp.exp(b_last - b)
        a = jnp.where(causal, jnp.einsum('bhtk,bhsk->bhts', q_dec, k_inv), 0.0)
        o = jnp.einsum('bhts,bhsv->bhtv', a, vc) + jnp.einsum('bhtk,bhkv->bhtv', q_dec, state)
        state = jnp.exp(b_last)[:, :, 0, :, None] * state + jnp.einsum('bhsk,bhsv->bhkv', k_end, vc)
        return state, o

    state0 = jnp.zeros((B, HG_HEADS, HG_EXPAND, HG_HEAD_V), jnp.float32)
    _, o = lax.scan(step, state0, (chunked(q), chunked(key), chunked(i), chunked(log_f)))
    return o.transpose(1, 0, 3, 2, 4).reshape(B, S, HG_HEADS, HG_HEAD_V)


def rms_norm_heads(o, gain):
    B, S, H, dv = o.shape
    o = o * lax.rsqrt(jnp.mean(jnp.square(o), -1, keepdims=True) + RMS_EPS)
    return (o * gain.astype(jnp.float32).reshape(H, dv)).reshape(B, S, H * dv)


def swiglu(h, w_gate, w_up, w_down):
    return (jax.nn.silu(h @ w_gate) * (h @ w_up)) @ w_down


def route(h, w_router, bias):
    T = h.shape[0]
    s = jax.nn.sigmoid(h.astype(jnp.float32) @ w_router.astype(jnp.float32))
    sel = s + bias.astype(jnp.float32)
    grp = sel.reshape(T, N_EXPERT_GROUPS, N_EXPERTS // N_EXPERT_GROUPS)
    grp_score = lax.top_k(grp, 2)[0].sum(-1)
    _, top_grp = lax.top_k(grp_score, TOPK_GROUPS)
    grp_mask = jnp.any(top_grp[..., :, None] == jnp.arange(N_EXPERT_GROUPS), axis=-2)
    expert_mask = jnp.repeat(grp_mask, N_EXPERTS // N_EXPERT_GROUPS, axis=-1)
    _, idx = lax.top_k(jnp.where(expert_mask, sel, -jnp.inf), TOP_K)
    g = jnp.take_along_axis(s, idx, axis=-1)
    g = g / g.sum(-1, keepdims=True) * ROUTED_SCALE
    return idx, g


def routed_experts(h, idx, gate, w_gate, w_up, w_down):
    T, D = h.shape
    A = T * TOP_K
    e_flat = idx.reshape(A).astype(jnp.int32)
    tok_flat = (jnp.arange(A, dtype=jnp.int32) // TOP_K)
    g_flat = gate.reshape(A)
    order = jnp.argsort(e_flat)
    e_s, tok_s, g_s = e_flat[order], tok_flat[order], g_flat[order]
    counts = jnp.bincount(e_flat, length=N_EXPERTS).astype(jnp.int32)
    padded = ((counts + MOE_BLOCK - 1) // MOE_BLOCK) * MOE_BLOCK
    start = jnp.cumsum(counts) - counts
    pend = jnp.cumsum(padded)
    pstart = pend - padded
    dest = pstart[e_s] + (jnp.arange(A, dtype=jnp.int32) - start[e_s])
    n_blocks = -(-(A + N_EXPERTS * (MOE_BLOCK - 1)) // MOE_BLOCK)
    P = n_blocks * MOE_BLOCK
    buf_tok = jnp.full((P,), T, jnp.int32).at[dest].set(tok_s)
    buf_g = jnp.zeros((P,), h.dtype).at[dest].set(g_s.astype(h.dtype))
    block_expert = jnp.minimum(
        jnp.searchsorted(pend, jnp.arange(n_blocks, dtype=jnp.int32) * MOE_BLOCK, side='right'),
        N_EXPERTS - 1).astype(jnp.int32)
    h_pad = jnp.concatenate([h, jnp.zeros((1, D), h.dtype)], axis=0)

    def step(acc, blk):
        tok_b, g_b, e = blk
        xb = h_pad[tok_b]
        a = jax.nn.silu(xb @ w_gate[e]) * (xb @ w_up[e])
        out = (a @ w_down[e]) * g_b[:, None]
        return acc.at[tok_b].add(out.astype(acc.dtype)), None

    acc, _ = lax.scan(step, jnp.zeros((T + 1, D), h.dtype),
                      (buf_tok.reshape(n_blocks, MOE_BLOCK), buf_g.reshape(n_blocks, MOE_BLOCK), block_expert))
    return acc[:T]


def setup_inputs(seed: int = 0) -> dict:
    key = jax.random.key(seed)
    ks = jax.random.split(key, 24)
    f32 = jnp.float32
    beta = DEEPNORM_BETA

    def nrm(k, shape, fan_in, scale=1.0):
        return jax.random.normal(k, shape, f32) * (fan_in ** -0.5 * scale)

    col_scale = np.ones((N_IN,), np.float32)
    v0 = 2 * ATT_QK_WIDTH
    col_scale[v0:v0 + ATT_QK_WIDTH] = beta
    i0 = 3 * ATT_QK_WIDTH + 2 * HG_WIDTH
    col_scale[i0:i0 + HG_WIDTH] = beta

    return {
        'x': jax.random.normal(ks[0], (BATCH, SEQ, D_MODEL), f32),
        'p': jax.random.normal(ks[1], (DEPTH, BATCH, SEQ, PLE_DIM), f32),
        'w_in': nrm(ks[2], (DEPTH, D_MODEL, N_IN), D_MODEL) * jnp.asarray(col_scale),
        'hgrn_lb_logits': 0.1 * jax.random.normal(ks[3], (DEPTH + 1, HG_WIDTH), f32),
        'hgrn_norm_w': 1.0 + 0.05 * jax.random.normal(ks[4], (DEPTH, HG_WIDTH), f32),
        'w_branch_att': nrm(ks[5], (DEPTH, ATT_OUT_WIDTH, D_MODEL), ATT_OUT_WIDTH, beta),
        'w_branch_hgrn': nrm(ks[6], (DEPTH, HG_WIDTH, D_MODEL), HG_WIDTH, beta),
        'w_out': nrm(ks[7], (DEPTH, D_MODEL, D_MODEL), D_MODEL, beta),
        'ln1_w': 1.0 + 0.05 * jax.random.normal(ks[8], (DEPTH, D_MODEL), f32),
        'ln1_b': 0.02 * jax.random.normal(ks[9], (DEPTH, D_MODEL), f32),
        'router_w': nrm(ks[10], (DEPTH, D_MODEL, N_EXPERTS), D_MODEL),
        'router_bias': 0.01 * jax.random.normal(ks[11], (DEPTH, N_EXPERTS), f32),
        'expert_w_gate': nrm(ks[12], (DEPTH, N_EXPERTS, D_MODEL, EXPERT_FF), D_MODEL),
        'expert_w_up': nrm(ks[13], (DEPTH, N_EXPERTS, D_MODEL, EXPERT_FF), D_MODEL, beta),
        'expert_w_down': nrm(ks[14], (DEPTH, N_EXPERTS, EXPERT_FF, D_MODEL), EXPERT_FF, beta),
        'shared_w_gate': nrm(ks[15], (DEPTH, D_MODEL, SHARED_FF), D_MODEL),
        'shared_w_up': nrm(ks[16], (DEPTH, D_MODEL, SHARED_FF), D_MODEL, beta),
        'shared_w_down': nrm(ks[17], (DEPTH, SHARED_FF, D_MODEL), SHARED_FF, beta),
        'ple_gate_w': nrm(ks[18], (DEPTH, D_MODEL, D_MODEL), D_MODEL),
        'ple_proj_w': nrm(ks[19], (DEPTH, PLE_DIM, D_MODEL), PLE_DIM, beta),
        'ln2_w': 1.0 + 0.05 * jax.random.normal(ks[20], (DEPTH, D_MODEL), f32),
        'ln2_b': 0.02 * jax.random.normal(ks[21], (DEPTH, D_MODEL), f32),
    }


def reference(x, p, w_in, hgrn_lb_logits, hgrn_norm_w, w_branch_att, w_branch_hgrn, w_out,
              ln1_w, ln1_b, router_w, router_bias, expert_w_gate, expert_w_up, expert_w_down,
              shared_w_gate, shared_w_up, shared_w_down, ple_gate_w, ple_proj_w, ln2_w, ln2_b):
    B, S, D = x.shape
    split_points = np.cumsum(SPLIT_SIZES)[:-1].tolist()
    lower_bounds = jnp.cumsum(jax.nn.softmax(hgrn_lb_logits.astype(jnp.float32), axis=0), axis=0)

    def att_heads(t):
        return t.reshape(B, S, N_ATT_GROUPS, ATT_HEADS, ATT_HEAD_DIM)

    for l in range(DEPTH):
        u = x @ w_in[l]
        q_a, k_a, v_a, q_h, f_h, i_h, g_h, gate_a, gate_h = jnp.split(u, split_points, axis=-1)
        y_att = dilated_attention(att_heads(q_a), att_heads(k_a), att_heads(v_a))
        y_att = y_att.reshape(B, S, ATT_OUT_WIDTH).astype(x.dtype)
        o_h = hgrn2_recurrence(q_h, f_h, jax.nn.silu(i_h), lower_bounds[l])
        y_hg = (rms_norm_heads(o_h, hgrn_norm_w[l]) * jax.nn.silu(g_h.astype(jnp.float32))).astype(x.dtype)
        merged = (jax.nn.sigmoid(gate_a) * (y_att @ w_branch_att[l])
                  + jax.nn.sigmoid(gate_h) * (y_hg @ w_branch_hgrn[l]))
        x1 = layer_norm(DEEPNORM_ALPHA * x + merged @ w_out[l], ln1_w[l], ln1_b[l])

        h = x1.reshape(B * S, D)
        idx, gate = route(h, router_w[l], router_bias[l])
        y_moe = (routed_experts(h, idx, gate, expert_w_gate[l], expert_w_up[l], expert_w_down[l])
                 + swiglu(h, shared_w_gate[l], shared_w_up[l], shared_w_down[l]))
        ple = jax.nn.sigmoid(x1 @ ple_gate_w[l]) * (p[l] @ ple_proj_w[l])
        x = layer_norm(DEEPNORM_ALPHA * x1 + y_moe.reshape(B, S, D) + ple, ln2_w[l], ln2_b[l])
    return x
```

```python
import numpy as np
import ml_dtypes
from contextlib import ExitStack
import concourse.bass as bass
import concourse.mybir as mybir
from concourse.bass_utils import run_bass_kernel_spmd

F32 = mybir.dt.float32
BF16 = mybir.dt.bfloat16
AF = mybir.ActivationFunctionType
ALU = mybir.AluOpType

T = 4096
D = 1024
NT = 32
NIN = 10752
QA0, KA0, VA0, QH0, FH0, IH0, GH0, GA0, GHG0 = 0, 1536, 3072, 4608, 5632, 6656, 7680, 8704, 9728
DILS = (1, 4, 16)
ALPHA = float(2.0 ** 0.25)
LN_EPS = 1e-5
RMS_EPS = 1e-6
NE = 64
NEG = -30000.0


class Res:
    __slots__ = ("name", "w", "rs", "excl")

    def __init__(self, name="", excl=False):
        self.name = name
        self.w = None
        self.rs = {}
        self.excl = excl


def PBanks():
    return [Res("bank%d" % i, excl=True) for i in range(8)]


class Sched:
    def __init__(self, nc, plan=None):
        self.nc = nc
        self.engs = {"pe": nc.tensor, "act": nc.scalar, "dve": nc.vector, "pool": nc.gpsimd, "sp": nc.sync}
        self.sems = {}
        self.count = {}
        self.known = {k: {} for k in self.engs}
        self.nins = 0
        self.plan = plan
        self.needed = {}
        self.defer = None

    def sem(self, name):
        if name not in self.sems:
            self.sems[name] = self.nc.alloc_semaphore(name="s_" + name)
            self.count[name] = 0
        return self.sems[name]

    def _wait(self, eng, s, v):
        self.needed.setdefault(s, set()).add(v)
        if self.plan is not None and s.startswith("e_"):
            v = self.plan[s][v]
        self.engs[eng].wait_ge(self.sem(s), v)

    def op(self, eng, fn, reads=(), writes=(), dma=None):
        if self.defer is not None:
            self.defer.append((eng, fn, tuple(reads), tuple(writes), dma))
            return None
        ex = [r for r in reads if r.excl and r not in writes]
        if ex:
            writes = tuple(writes) + tuple(ex)
        deps = {}
        own = "e_" + eng

        def add(tok):
            if tok is None:
                return
            s, v = tok
            if deps.get(s, 0) < v:
                deps[s] = v

        for r in reads:
            add(r.w)
        for w in writes:
            add(w.w)
            for s, v in w.rs.items():
                add((s, v))
        e = self.engs[eng]
        kn = self.known[eng]
        for s, v in deps.items():
            if eng == "pe" and s == own:
                continue
            if kn.get(s, 0) < v:
                self._wait(eng, s, v)
                kn[s] = v
        if dma is None:
            s = own
            inc = 1
        else:
            s = "d_" + dma
            inc = 16
        h = self.sem(s)
        self.count[s] += inc
        tok = (s, self.count[s])
        ins = fn(e)
        if dma is not None or self.plan is None or tok[1] in self.plan.get(s, {}):
            ins.then_inc(h, inc)
        self.nins += 1
        for r in reads:
            if r.rs.get(s, 0) < tok[1]:
                r.rs[s] = tok[1]
        for w in writes:
            w.w = tok
            w.rs = {}
        return tok

    def barrier(self):
        for eng in self.engs:
            kn = self.known[eng]
            for s, v in self.count.items():
                if v > 0 and kn.get(s, 0) < v:
                    self._wait(eng, s, v)
                    kn[s] = v

    def make_plan(self):
        return {s: {v: i + 1 for i, v in enumerate(sorted(vs))} for s, vs in self.needed.items()}


def make_consts():
    bf = ml_dtypes.bfloat16
    c = {}
    c["c_ident_bf"] = np.eye(128, dtype=np.float32).astype(bf)
    c["c_ident_f"] = np.eye(128, dtype=np.float32)
    k = np.arange(128)[:, None].astype(np.float64)
    q = np.arange(128)[None, :].astype(np.float64)
    ab = np.zeros((128, 48, 128), np.float32)
    for g, d in enumerate(DILS):
        for h in range(8):
            slope = 2.0 ** (-(h + 1))
            cur = np.where(k <= q, -slope * d * (q - k), NEG)
            prev = np.where(k >= q, -slope * d * (q + 128 - k), NEG)
            ab[:, (g * 8 + h) * 2 + 0, :] = cur
            ab[:, (g * 8 + h) * 2 + 1, :] = prev
    c["c_abias"] = ab.reshape(128, 48 * 128).astype(bf)
    c["c_ones_bf"] = np.ones((128, 64), np.float32).astype(bf)
    s = np.arange(128)[:, None]
    t = np.arange(128)[None, :]
    same = (s // 64) == (t // 64)
    c["c_tri_incl"] = (same & (s <= t)).astype(np.float32)
    c["c_tri_after"] = (same & (s > t)).astype(np.float32)
    sel = np.zeros((128, 2), np.float32)
    sel[63, 0] = 1.0
    sel[127, 1] = 1.0
    c["c_sel2"] = sel
    cm = np.zeros((128, 2), np.float32)
    cm[:64, 0] = 1.0
    cm[64:, 1] = 1.0
    c["c_cm"] = cm
    return c


def build_program(stop_after="D", dbg=(), skip=(), small=None, inject=()):
    _, _, S1 = _build(stop_after, dbg, skip, small, None, inject)
    nc, name, S2 = _build(stop_after, dbg, skip, small, S1.make_plan(), inject)
    return nc, name


def _build(stop_after, dbg, skip, small, plan, inject=()):
    nc = bass.Bass("TRN2", target_bir_lowering=False)

    def din(name, shape, dt=F32):
        return nc.dram_tensor(name, list(shape), dt, kind="ExternalInput").ap()

    def dscr(name, shape, dt):
        if name in inject:
            return nc.dram_tensor(name, list(shape), dt, kind="ExternalInput").ap()
        if name in dbg:
            return nc.dram_tensor(name, list(shape), dt, kind="ExternalOutput").ap()
        return nc.dram_tensor(name, list(shape), dt).ap()

    x_d = din("x", [T, D])
    p_d = din("p", [T, 256])
    w_in = din("w_in", [D, NIN])
    w_a = din("w_a", [512, D])
    w_h = din("w_h", [D, D])
    w_o = din("w_o", [D, D])
    w_r = din("w_r", [D, NE])
    ewg = din("ewg", [NE, D, 256])
    ewu = din("ewu", [NE, D, 256])
    ewd = din("ewd", [NE, 256, D])
    swg = din("swg", [D, 256])
    swu = din("swu", [D, 256])
    swd = din("swd", [256, D])
    w_pg = din("w_pg", [D, D])
    w_pp = din("w_pp", [256, D])
    v_lb0 = din("v_lb0", [128, D])
    v_lb1 = din("v_lb1", [128, D])
    v_gain = din("v_gain", [128, D])
    v_ln1w = din("v_ln1w", [128, D])
    v_ln1b = din("v_ln1b", [128, D])
    v_ln2w = din("v_ln2w", [128, D])
    v_ln2b = din("v_ln2b", [128, D])
    v_rb = din("v_rb", [128, NE])
    c_ident_bf = din("c_ident_bf", [128, 128], BF16)
    c_ident_f = din("c_ident_f", [128, 128])
    c_abias = din("c_abias", [128, 48 * 128], BF16)
    c_ones_bf = din("c_ones_bf", [128, 64], BF16)
    c_tri_incl = din("c_tri_incl", [128, 128])
    c_tri_after = din("c_tri_after", [128, 128])
    c_sel2 = din("c_sel2", [128, 2])
    c_cm = din("c_cm", [128, 2])
    out_d = nc.dram_tensor("out", [T, D], F32, kind="ExternalOutput").ap()

    xT_d = dscr("xT_d", [8, 128, T], BF16)
    yattT_d = dscr("yattT_d", [4, 128, T], BF16)
    yhgT_d = dscr("yhgT_d", [8, 128, T], BF16)
    x1T_d = dscr("x1T_d", [8, 128, T], BF16)
    x1_d = dscr("x1_d", [T, D], F32)
    G_d = dscr("G_d", [T, NE], F32)
    wg_d = dscr("wg_d", [NE + 1, 128, 8 * 256], BF16)
    wu_d = dscr("wu_d", [NE + 1, 128, 8 * 256], BF16)
    wd_d = dscr("wd_d", [NE + 1, 128, 2 * 1024], BF16)

    S = Sched(nc, plan)
    op = S.op
    ps = nc.alloc_psum_tensor("ps", [128, 4096], F32).ap()

    def bank(b):
        return ps[:, b * 512:(b + 1) * 512]

    def bankbf(b):
        return bank(b).bitcast(BF16)

    R_xTd, R_yattd, R_yhgd, R_x1Td, R_x1d, R_Gd, R_wcast = [Res(n) for n in "xTd yattd yhgd x1Td x1d Gd wcast".split()]

    with ExitStack() as esg:
        def sbg(name, shape, dt):
            return esg.enter_context(nc.sbuf_tensor(name, list(shape), dt)).ap()

        ident = sbg("ident", [128, 128], BF16)
        identf = sbg("identf", [128, 128], F32)
        R_c = Res("consts")
        op("sp", lambda e: e.dma_start(out=ident[:], in_=c_ident_bf[:, :]), writes=[R_c], dma="c0")
        op("sp", lambda e: e.dma_start(out=identf[:], in_=c_ident_f[:, :]), writes=[R_c], dma="c1")
        S.barrier()

        castq = []
        for e_ in range(NE + 1):
            sg_, su_, sd_ = (ewg[e_], ewu[e_], ewd[e_]) if e_ < NE else (swg, swu, swd)
            castq.append((wg_d[e_].rearrange("p (k j) -> p k j", k=8), sg_.rearrange("(k p) j -> p k j", p=128)))
            castq.append((wu_d[e_].rearrange("p (k j) -> p k j", k=8), su_.rearrange("(k p) j -> p k j", p=128)))
            castq.append((wd_d[e_].rearrange("p (k j) -> p k j", k=2), sd_.rearrange("(k p) j -> p k j", p=128)))

        def issue_casts(n):
            for _ in range(n):
                if castq:
                    o_, i_ = castq.pop(0)
                    op("pool", lambda e, o_=o_, i_=i_: e.dma_start(out=o_, in_=i_), writes=[R_wcast], dma="wcast")

        with ExitStack() as esx:
            xT = esx.enter_context(nc.sbuf_tensor("xT", [128, 8, T], BF16)).ap()
            R_xT = Res("xT")
            with ExitStack() as es0:
                xb = [es0.enter_context(nc.sbuf_tensor(f"xb{i}", [128, D], BF16)).ap() for i in range(2)]
                R_xb = [Res(), Res()]
                R_pb = [Res(excl=True), Res(excl=True)]
                for t in range(0 if "0" in skip else NT):
                    i = t % 2
                    op("pool", lambda e, t=t, i=i: e.dma_start(out=xb[i][:], in_=x_d[t * 128:(t + 1) * 128, :]),
                       writes=[R_xb[i]], dma=f"xb{i}")
                    pb = bankbf(i)
                    for k in range(8):
                        op("pe", lambda e, k=k, i=i, pb=pb: e.transpose(out=pb[:, k * 128:(k + 1) * 128], in_=xb[i][:, k * 128:(k + 1) * 128], identity=ident[:]),
                           reads=[R_xb[i], R_c], writes=[R_pb[i]])
                    eng = "act" if i else "dve"
                    if eng == "act":
                        op("act", lambda e, t=t, pb=pb: e.copy(out=xT[:, :, t * 128:(t + 1) * 128], in_=pb.rearrange("p (k j) -> p k j", k=8)),
                           reads=[R_pb[i]], writes=[R_xT])
                    else:
                        op("dve", lambda e, t=t, pb=pb: e.tensor_copy(out=xT[:, :, t * 128:(t + 1) * 128], in_=pb.rearrange("p (k j) -> p k j", k=8)),
                           reads=[R_pb[i]], writes=[R_xT])
                for k in range(0 if "0" in skip else 8):
                    op("sp", lambda e, k=k: e.dma_start(out=xT_d[k], in_=xT[:, k, :]), reads=[R_xT], writes=[R_xTd], dma="xTd")
                S.barrier()

            with ExitStack() as esa:
                def sba(name, shape, dt):
                    return esa.enter_context(nc.sbuf_tensor(name, list(shape), dt)).ap()

                abias = sba("abias", [128, 48 * 128], BF16)
                ones_bf = sba("ones_bf", [128, 64], BF16)
                R_ca = Res()
                op("sp", lambda e: e.dma_start(out=abias[:], in_=c_abias[:, :]), writes=[R_ca], dma="c0")
                op("sp", lambda e: e.dma_start(out=ones_bf[:], in_=c_ones_bf[:, :]), writes=[R_ca], dma="c1")
                wq = [sba(f"wq{i}", [128, 8, 128], BF16) for i in range(2)]
                wk = [sba(f"wk{i}", [128, 8, 128], BF16) for i in range(2)]
                wv = [sba(f"wv{i}", [128, 8, 128], BF16) for i in range(2)]
                R_w = [Res(), Res()]
                QT = sba("QT", [128, T], BF16)
                KTm = [sba("KTm0", [128, T], BF16), sba("KTm1", [128, T], BF16)]
                Vb = sba("Vb", [128, 32, 128], BF16)
                R_QT, R_KT, R_Vb = Res(), Res(), Res()
                accN = sba("accN", [128, T], F32)
                accD = sba("accD", [128, T], F32)
                R_acc = Res()
                ybuf = sba("ybuf", [128, T], BF16)
                R_yb = Res()
                PT = [sba(f"PT{i}", [128, 512], BF16) for i in range(4)]
                R_PT = [Res() for _ in range(4)]
                RB = PBanks()
                op("pool", lambda e: e.memset(KTm[0][64:128, :], 0.0), writes=[R_KT])
                op("pool", lambda e: e.memset(KTm[1][0:64, :], 0.0), writes=[R_KT])
                S.barrier()

                its = [(hp, g) for hp in range(4) for g in range(3)]
                if "A" in skip:
                    its = []
                if small and "AITS" in small:
                    its = its[:small["AITS"]]

                def load_w(idx):
                    hp, g = its[idx]
                    i = idx % 2
                    col = g * 512 + hp * 128
                    for wt, base in ((wq[i], QA0), (wk[i], KA0), (wv[i], VA0)):
                        op("pool", lambda e, wt=wt, c0=base + col: e.dma_start(out=wt[:], in_=w_in[:, c0:c0 + 128].rearrange("(k p) j -> p k j", p=128)),
                           writes=[R_w[i]], dma=f"aw{i}")

                if its:
                    load_w(0)
                for idx, (hp, g) in enumerate(its):
                    i = idx % 2
                    d = DILS[g]
                    if idx + 1 < len(its):
                        load_w(idx + 1)

                    def cols(blk, d=d):
                        n, r = blk // d, blk % d
                        return slice(n * 128 * d + r, (n + 1) * 128 * d, d)

                    for s in range(8):
                        bq, bk = 6, 7
                        for kc in range(8):
                            op("pe", lambda e, kc=kc, s=s, i=i: e.matmul(bank(6), lhsT=wq[i][:, kc, :], rhs=xT[:, kc, s * 512:(s + 1) * 512], start=(kc == 0), stop=(kc == 7)),
                               reads=[R_w[i], R_xT], writes=[RB[6]])
                        op("act", lambda e, s=s: e.mul(out=QT[:, s * 512:(s + 1) * 512], in_=bank(6), mul=0.125),
                           reads=[RB[6]], writes=[R_QT])
                        for kc in range(8):
                            op("pe", lambda e, kc=kc, s=s, i=i: e.matmul(bank(7), lhsT=wk[i][:, kc, :], rhs=xT[:, kc, s * 512:(s + 1) * 512], start=(kc == 0), stop=(kc == 7)),
                               reads=[R_w[i], R_xT], writes=[RB[7]])
                        op("dve", lambda e, s=s: e.tensor_copy(out=KTm[0][0:64, s * 512:(s + 1) * 512], in_=bank(7)[0:64, :]),
                           reads=[RB[7]], writes=[R_KT])
                        op("dve", lambda e, s=s: e.tensor_copy(out=KTm[1][64:128, s * 512:(s + 1) * 512], in_=bank(7)[64:128, :]),
                           reads=[RB[7]], writes=[R_KT])
                    for j in range(8):
                        b_ = 6 + (j % 2)
                        for bb in range(4):
                            blk = 4 * j + bb
                            for kc in range(8):
                                op("pe", lambda e, kc=kc, bb=bb, blk=blk, b_=b_, i=i: e.matmul(bank(b_)[:, bb * 128:(bb + 1) * 128], lhsT=xT[:, kc, cols(blk)], rhs=wv[i][:, kc, :], start=(kc == 0), stop=(kc == 7)),
                                   reads=[R_w[i], R_xT], writes=[RB[b_]])
                        if j % 2:
                            op("act", lambda e, j=j, b_=b_: e.copy(out=Vb[:, 4 * j:4 * j + 4, :], in_=bank(b_).rearrange("p (a q) -> p a q", a=4)),
                               reads=[RB[b_]], writes=[R_Vb])
                        else:
                            op("dve", lambda e, j=j, b_=b_: e.tensor_copy(out=Vb[:, 4 * j:4 * j + 4, :], in_=bank(b_).rearrange("p (a q) -> p a q", a=4)),
                               reads=[RB[b_]], writes=[R_Vb])

                    def stage1(blk):
                        hasprev = blk >= d
                        sb_ = blk % 4
                        bi = ((g * 8 + hp * 2) * 2) * 128
                        if hasprev:
                            op("pe", lambda e, bi=bi: e.matmul(bank(sb_), lhsT=ident[:], rhs=abias[:, bi:bi + 512], start=True, stop=False),
                               reads=[R_ca, R_c], writes=[RB[sb_]])
                        for a in range(2):
                            for c in range(2 if hasprev else 1):
                                kblk = blk - c * d
                                o_ = bank(sb_)[:, (a * 2 + c) * 128:(a * 2 + c + 1) * 128]
                                if not hasprev:
                                    bi2 = bi + (a * 2 + c) * 128
                                    op("pe", lambda e, o_=o_, bi2=bi2: e.matmul(o_, lhsT=ident[:], rhs=abias[:, bi2:bi2 + 128], start=True, stop=False),
                                       reads=[R_ca, R_c], writes=[RB[sb_]])
                                op("pe", lambda e, o_=o_, a=a, kblk=kblk, blk=blk: e.matmul(o_, lhsT=KTm[a][:, cols(kblk)], rhs=QT[:, cols(blk)], start=False, stop=((not hasprev) or (a == 1 and c == 1))),
                                   reads=[R_KT, R_QT], writes=[RB[sb_]])
                        if hasprev:
                            op("act", lambda e, sb_=sb_: e.activation(out=PT[sb_][:], in_=bank(sb_), func=AF.Exp),
                               reads=[RB[sb_]], writes=[R_PT[sb_]])
                        else:
                            op("act", lambda e, sb_=sb_: e.activation(out=PT[sb_].rearrange("p (a c q) -> p a c q", a=2, c=2)[:, :, 0, :],
                                                                      in_=bank(sb_).rearrange("p (a c q) -> p a c q", a=2, c=2)[:, :, 0, :], func=AF.Exp),
                               reads=[RB[sb_]], writes=[R_PT[sb_]])

                    def stage2(blk):
                        hasprev = blk >= d
                        sb_ = blk % 4
                        q_ = blk % 2
                        bnd = 4 + (blk // 2) % 2
                        for a in range(2):
                            for isden in range(2):
                                for c in range(2 if hasprev else 1):
                                    kblk = blk - c * d
                                    rhs_ = PT[sb_][:, (a * 2 + c) * 128:(a * 2 + c + 1) * 128]
                                    if not isden:
                                        op("pe", lambda e, a=a, c=c, kblk=kblk, rhs_=rhs_: e.matmul(bank(bnd)[a * 64:(a + 1) * 64, q_ * 128:(q_ + 1) * 128], lhsT=Vb[:, kblk, a * 64:(a + 1) * 64], rhs=rhs_, start=(c == 0), stop=(c == 1 or not hasprev)),
                                           reads=[R_Vb, R_PT[sb_]], writes=[RB[bnd]])
                                    else:
                                        op("pe", lambda e, a=a, c=c, rhs_=rhs_: e.matmul(bank(bnd)[a * 64:(a + 1) * 64, 256 + q_ * 128:256 + (q_ + 1) * 128], lhsT=ones_bf[:, :], rhs=rhs_, start=(c == 0), stop=(c == 1 or not hasprev)),
                                           reads=[R_ca, R_PT[sb_]], writes=[RB[bnd]])
                        if q_ == 1:
                            blk0 = blk - 1
                            if d == 1:
                                def view(acc):
                                    return acc[:, blk0 * 128:(blk0 + 2) * 128].rearrange("p (a q) -> p a q", a=2)
                            else:
                                n, r0 = blk0 // d, blk0 % d

                                def view(acc, n=n, r0=r0):
                                    return acc[:, n * 128 * d:(n + 1) * 128 * d].rearrange("p (q r) -> p r q", r=d)[:, r0:r0 + 2, :]
                            for acc, c0 in ((accN, 0), (accD, 256)):
                                pv = bank(bnd)[:, c0:c0 + 256].rearrange("p (a q) -> p a q", a=2)
                                if g == 0:
                                    op("dve", lambda e, acc=acc, pv=pv: e.tensor_copy(out=view(acc), in_=pv), reads=[RB[bnd]], writes=[R_acc])
                                else:
                                    op("dve", lambda e, acc=acc, pv=pv: e.tensor_tensor(out=view(acc), in0=view(acc), in1=pv, op=ALU.add), reads=[RB[bnd], R_acc], writes=[R_acc])

                    LA = 3
                    for blk in range(32 + LA):
                        if blk >= LA:
                            stage2(blk - LA)
                        if blk < 32:
                            stage1(blk)
                        issue_casts(1)
                    if g == 2:
                        op("dve", lambda e: e.reciprocal(out=accD[:], in_=accD[:]), reads=[R_acc], writes=[R_acc])
                        op("dve", lambda e: e.tensor_tensor(out=ybuf[:], in0=accN[:], in1=accD[:], op=ALU.mult), reads=[R_acc], writes=[R_yb])
                        op("sp", lambda e, hp=hp: e.dma_start(out=yattT_d[hp], in_=ybuf[:]), reads=[R_yb], writes=[R_yattd], dma="yatt")
                S.barrier()
            if stop_after == "A":
                return nc, "yattT_d", S

            with ExitStack() as esb:
                def sbb(name, shape, dt):
                    return esb.enter_context(nc.sbuf_tensor(name, list(shape), dt)).ap()

                tri_incl = sbb("tri_incl", [128, 128], F32)
                tri_after = sbb("tri_after", [128, 128], F32)
                sel2 = sbb("sel2", [128, 2], F32)
                cmk = sbb("cmk", [128, 2], F32)
                lbt = sbb("lbt", [128, D], F32)
                omlt = sbb("omlt", [128, D], F32)
                gaint = sbb("gaint", [128, D], F32)
                R_cb = Res()
                op("sp", lambda e: e.dma_start(out=tri_incl[:], in_=c_tri_incl[:, :]), writes=[R_cb], dma="c0")
                op("sp", lambda e: e.dma_start(out=tri_after[:], in_=c_tri_after[:, :]), writes=[R_cb], dma="c1")
                op("sp", lambda e: e.dma_start(out=sel2[:], in_=c_sel2[:, :]), writes=[R_cb], dma="c2")
                op("sp", lambda e: e.dma_start(out=cmk[:], in_=c_cm[:, :]), writes=[R_cb], dma="c6")
                op("sp", lambda e: e.dma_start(out=lbt[:], in_=v_lb0[:, :]), writes=[R_cb], dma="c3")
                op("sp", lambda e: e.dma_start(out=omlt[:], in_=v_lb1[:, :]), writes=[R_cb], dma="c4")
                op("sp", lambda e: e.dma_start(out=gaint[:], in_=v_gain[:, :]), writes=[R_cb], dma="c5")
                S.barrier()
                op("dve", lambda e: e.tensor_tensor(out=omlt[:], in0=omlt[:], in1=lbt[:], op=ALU.subtract), reads=[R_cb], writes=[R_cb])
                op("act", lambda e: e.activation(out=omlt[:], in_=omlt[:], func=AF.Exp), reads=[R_cb], writes=[R_cb])
                op("dve", lambda e: e.tensor_scalar_add(out=omlt[:], in0=omlt[:], scalar1=1.0), reads=[R_cb], writes=[R_cb])
                op("dve", lambda e: e.reciprocal(out=lbt[:], in_=omlt[:]), reads=[R_cb], writes=[R_cb])
                op("dve", lambda e: e.tensor_scalar(out=omlt[:], in0=lbt[:], scalar1=-1.0, scalar2=1.0, op0=ALU.mult, op1=ALU.add), reads=[R_cb], writes=[R_cb])
                S.barrier()

                whp = [sbb(f"whp{i}", [128, 8, 4, 256], BF16) for i in range(2)]
                R_whp = [Res(), Res()]
                YT = [sbb("YT0", [128, 2, T], BF16)] * 2
                R_YT = [Res()] * 2

                def mk(name, shape, dt, n):
                    return [sbb(f"{name}{i}", shape, dt) for i in range(n)], [Res() for _ in range(n)]

                Eb, R_Eb = mk("Eb", [128, 768], F32, 2)
                FG, R_FG = mk("FG", [128, 256], F32, 2)
                LF, R_LF = mk("LF", [128, 256], F32, 2)
                KEY, R_KEY = mk("KEY", [128, 256], F32, 2)
                Vv, R_V = mk("Vv", [128, 256], BF16, 5)
                SG, R_SG = mk("SG", [128, 256], F32, 5)
                EB, R_EB = mk("EB", [128, 512], F32, 2)
                ENB, R_ENB = mk("ENB", [128, 256], F32, 2)
                QD, R_QD = mk("QD", [128, 256], BF16, 2)
                KI, R_KI = mk("KI", [128, 256], BF16, 2)
                KE, R_KE = mk("KE", [128, 2, 256], BF16, 3)
                KEF, R_KEF = mk("KEF", [128, 256], F32, 2)
                DEC, R_DEC = mk("DEC", [128, 4], F32, 3)
                QKT, R_QKT = mk("QKT", [128, 4, 128], BF16, 3)
                AT, R_AT = mk("AT", [128, 2, 128], BF16, 3)
                YH, R_YH = mk("YH", [128, 256], BF16, 2)
                Y1, R_Y1 = mk("Y1", [128, 256], F32, 2)
                SS, R_SS = mk("SS", [128, 4], F32, 2)
                junk = sbb("junk", [128, 128], F32)
                R_junk = Res()
                ST = [sbb(f"ST{a}", [128, 128], F32) for a in range(2)]
                R_ST = [Res(), Res()]
                SBF = [[[sbb(f"SBF{a}{c}{u}", [128, 128], BF16) for u in range(3)] for c in range(2)] for a in range(2)]
                R_SBF = [[[Res() for u in range(3)] for c in range(2)] for a in range(2)]
                RB = PBanks()
                R_PB, R_U, R_TRqk, R_TRy, R_DECp, R_aT, R_o = RB[4], RB[5], RB[6], RB[6], RB[6], RB[7], RB[7]

                def PJ(i):
                    return ps[:, i * 1024:(i + 1) * 1024]

                def load_whp(hp):
                    i = hp % 2
                    for ty, base in enumerate((QH0, FH0, IH0, GH0)):
                        c0 = base + hp * 256
                        op("pool", lambda e, i=i, ty=ty, c0=c0: e.dma_start(out=whp[i][:, :, ty, :], in_=w_in[:, c0:c0 + 256].rearrange("(k p) j -> p k j", p=128)),
                           writes=[R_whp[i]], dma=f"whp{i}")

                if "B" not in skip:
                    load_whp(0)
                NTB = small["NTB"] if small and "NTB" in small else NT
                for hp in range(0 if "B" in skip else (small["HPS"] if small and "HPS" in small else 4)):
                    wi = hp % 2
                    if hp + 1 < 4:
                        load_whp(hp + 1)
                    hs = slice(hp * 256, (hp + 1) * 256)
                    for a in range(2):
                        op("dve", lambda e, a=a: e.memset(ST[a][:], 0.0), writes=[R_ST[a]])
                        op("dve", lambda e, a=a: e.memset(SBF[a][0][0][:], 0.0), writes=[R_SBF[a][0][0]])

                    def s0(t):
                        i = t % 2
                        for half in range(2):
                            for kc in range(8):
                                op("pe", lambda e, kc=kc, half=half, i=i, t=t: e.matmul(PJ(i)[:, half * 512:(half + 1) * 512], lhsT=xT[:, kc, t * 128:(t + 1) * 128],
                                                                                     rhs=whp[wi][:, kc, 2 * half:2 * half + 2, :].rearrange("p a j -> p (a j)"), start=(kc == 0), stop=(kc == 7)),
                                   reads=[R_xT, R_whp[wi]], writes=[RB[2 * i + half]])

                    def s1(t):
                        i = t % 2
                        op("act", lambda e, i=i: e.activation(out=Eb[i][:], in_=PJ(i)[:, 256:1024], func=AF.Exp, scale=-1.0), reads=[RB[2 * i], RB[2 * i + 1]], writes=[R_Eb[i]])
                        op("act", lambda e, i=i: e.activation(out=Eb[i][:], in_=Eb[i][:], func=AF.Ln, bias=1.0, scale=1.0), reads=[R_Eb[i]], writes=[R_Eb[i]])
                        op("act", lambda e, i=i: e.activation(out=Eb[i][:], in_=Eb[i][:], func=AF.Exp, scale=-1.0), reads=[R_Eb[i]], writes=[R_Eb[i]])
                        op("dve", lambda e, i=i: e.tensor_tensor(out=FG[i][:], in0=Eb[i][:, 0:256], in1=omlt[:, hs], op=ALU.mult), reads=[R_Eb[i], R_cb], writes=[R_FG[i]])
                        op("dve", lambda e, i=i: e.tensor_tensor(out=FG[i][:], in0=FG[i][:], in1=lbt[:, hs], op=ALU.add), reads=[R_FG[i], R_cb], writes=[R_FG[i]])
                        op("act", lambda e, i=i: e.activation(out=LF[i][:], in_=FG[i][:], func=AF.Ln), reads=[R_FG[i]], writes=[R_LF[i]])
                        op("dve", lambda e, i=i: e.tensor_scalar(out=KEY[i][:], in0=FG[i][:], scalar1=-1.0, scalar2=1.0, op0=ALU.mult, op1=ALU.add), reads=[R_FG[i]], writes=[R_KEY[i]])
                        op("dve", lambda e, i=i, t=t: e.tensor_tensor(out=Vv[t % 5][:], in0=PJ(i)[:, 512:768], in1=Eb[i][:, 256:512], op=ALU.mult), reads=[RB[2 * i + 1], R_Eb[i]], writes=[R_V[t % 5]])
                        op("dve", lambda e, i=i, t=t: e.tensor_tensor(out=SG[t % 5][:], in0=PJ(i)[:, 768:1024], in1=Eb[i][:, 512:768], op=ALU.mult), reads=[RB[2 * i + 1], R_Eb[i]], writes=[R_SG[t % 5]])

                    def s2(t):
                        i = t % 2
                        op("pe", lambda e, i=i: e.matmul(bank(4)[:, 0:256], lhsT=tri_incl[:], rhs=LF[i][:], start=True, stop=True), reads=[R_cb, R_LF[i]], writes=[R_PB])
                        op("pe", lambda e, i=i: e.matmul(bank(4)[:, 256:512], lhsT=tri_after[:], rhs=LF[i][:], start=True, stop=True), reads=[R_cb, R_LF[i]], writes=[R_PB])
                        op("act", lambda e, i=i: e.activation(out=EB[i][:], in_=bank(4), func=AF.Exp), reads=[R_PB], writes=[R_EB[i]])
                        op("act", lambda e, i=i: e.activation(out=ENB[i][:], in_=bank(4)[:, 0:256], func=AF.Exp, scale=-1.0), reads=[R_PB], writes=[R_ENB[i]])
                        op("dve", lambda e, i=i: e.tensor_tensor(out=QD[i][:], in0=PJ(i)[:, 0:256], in1=EB[i][:, 0:256], op=ALU.mult), reads=[RB[2 * i], R_EB[i]], writes=[R_QD[i]])
                        op("pool", lambda e, i=i: e.tensor_tensor(out=KI[i][:], in0=KEY[i][:], in1=ENB[i][:], op=ALU.mult), reads=[R_KEY[i], R_ENB[i]], writes=[R_KI[i]])
                        op("pool", lambda e, i=i: e.tensor_tensor(out=KEF[i][:], in0=KEY[i][:], in1=EB[i][:, 256:512], op=ALU.mult), reads=[R_KEY[i], R_EB[i]], writes=[R_KEF[i]])
                        for c in range(2):
                            op("act", lambda e, i=i, t=t, c=c: e.activation(out=KE[t % 3][:, c, :], in_=KEF[i][:], func=AF.Copy, scale=cmk[:, c:c + 1]), reads=[R_KEF[i], R_cb], writes=[R_KE[t % 3]])
                        for a in range(2):
                            op("pe", lambda e, i=i, a=a: e.matmul(bank(6)[:, 448 + 2 * a:450 + 2 * a], lhsT=EB[i][:, a * 128:(a + 1) * 128], rhs=sel2[:], start=True, stop=True), reads=[R_EB[i], R_cb], writes=[R_DECp])
                        op("dve", lambda e, t=t: e.tensor_copy(out=DEC[t % 3][:], in_=bank(6)[:, 448:452]), reads=[R_DECp], writes=[R_DEC[t % 3]])

                    def s3(t):
                        i = t % 2
                        j = t % 3
                        tb = bankbf(6)
                        for a in range(2):
                            op("pe", lambda e, i=i, a=a: e.transpose(out=tb[:, a * 128:(a + 1) * 128], in_=QD[i][:, a * 128:(a + 1) * 128], identity=ident[:]), reads=[R_QD[i], R_c], writes=[R_TRqk])
                            op("pe", lambda e, i=i, a=a: e.transpose(out=tb[:, (2 + a) * 128:(3 + a) * 128], in_=KI[i][:, a * 128:(a + 1) * 128], identity=ident[:]), reads=[R_KI[i], R_c], writes=[R_TRqk])
                        op("act", lambda e, j=j: e.copy(out=QKT[j][:], in_=tb[:, 0:512].rearrange("p (a q) -> p a q", a=4)), reads=[R_TRqk], writes=[R_QKT[j]])
                        for a in range(2):
                            op("pe", lambda e, j=j, a=a: e.matmul(bank(7)[:, a * 128:(a + 1) * 128], lhsT=QKT[j][:, 2 + a, :], rhs=QKT[j][:, a, :], start=True, stop=True), reads=[R_QKT[j]], writes=[R_aT])
                        for a in range(2):
                            op("dve", lambda e, j=j, a=a: e.tensor_tensor(out=AT[j][:, a, :], in0=bank(7)[:, a * 128:(a + 1) * 128], in1=tri_incl[:], op=ALU.mult), reads=[R_aT, R_cb], writes=[R_AT[j]])

                    def s4(t):
                        for a in range(2):
                            for c in range(2):
                                op("pe", lambda e, a=a, c=c, t=t: e.matmul(bank(5)[:, (a * 2 + c) * 128:(a * 2 + c + 1) * 128], lhsT=KE[t % 3][:, c, a * 128:(a + 1) * 128],
                                                                         rhs=Vv[t % 5][:, a * 128:(a + 1) * 128], start=True, stop=True),
                                   reads=[R_KE[t % 3], R_V[t % 5]], writes=[R_U])
                        lvl = small.get("S4", 3) if small else 3
                        for c in range(2 if lvl >= 2 else 0):
                            for a in range(2):
                                op("dve", lambda e, a=a, c=c, t=t: e.scalar_tensor_tensor(out=ST[a][:], in0=ST[a][:], scalar=DEC[t % 3][:, 2 * a + c:2 * a + c + 1], in1=bank(5)[:, (a * 2 + c) * 128:(a * 2 + c + 1) * 128], op0=ALU.mult, op1=ALU.add),
                                   reads=[R_ST[a], R_DEC[t % 3], R_U], writes=[R_ST[a]])
                                u_ = (t % 3) if c == 0 else ((t + 1) % 3)
                                if lvl < 3:
                                    continue
                                op("dve", lambda e, a=a, c=c, u_=u_: e.tensor_copy(out=SBF[a][1 - c][u_][:], in_=ST[a][:]), reads=[R_ST[a]], writes=[R_SBF[a][1 - c][u_]])

                    def s5(t):
                        j = t % 3
                        i = t % 2
                        for a in range(2):
                            o_ = bank(7)[:, 256 + a * 128:256 + (a + 1) * 128]
                            op("pe", lambda e, a=a, j=j, t=t, o_=o_: e.matmul(o_, lhsT=AT[j][:, a, :], rhs=Vv[t % 5][:, a * 128:(a + 1) * 128], start=True, stop=False), reads=[R_AT[j], R_V[t % 5]], writes=[R_o])
                            for c in range(2):
                                op("pe", lambda e, a=a, c=c, j=j, o_=o_, t=t: e.matmul(o_[c * 64:(c + 1) * 64, :], lhsT=QKT[j][:, a, c * 64:(c + 1) * 64], rhs=SBF[a][c][t % 3][:], start=False, stop=True),
                                   reads=[R_QKT[j], R_SBF[a][c][t % 3]], writes=[R_o])
                        op("pool", lambda e, i=i: e.memset(SS[i][:, 0:2], 0.0), writes=[R_SS[i]])
                        for a in range(2):
                            op("act", lambda e, a=a, i=i: e.activation(out=junk[:], in_=bank(7)[:, 256 + a * 128:256 + (a + 1) * 128], func=AF.Square, accum_out=SS[i][:, a:a + 1]), reads=[R_o], writes=[R_SS[i], R_junk])
                        op("act", lambda e, i=i: e.activation(out=SS[i][:, 2:4], in_=SS[i][:, 0:2], func=AF.Ln, scale=1.0 / 128.0, bias=RMS_EPS), reads=[R_SS[i]], writes=[R_SS[i]])
                        op("act", lambda e, i=i: e.activation(out=SS[i][:, 2:4], in_=SS[i][:, 2:4], func=AF.Exp, scale=-0.5), reads=[R_SS[i]], writes=[R_SS[i]])
                        for a in range(2):
                            op("dve", lambda e, a=a, i=i: e.scalar_tensor_tensor(out=Y1[i][:, a * 128:(a + 1) * 128], in0=bank(7)[:, 256 + a * 128:256 + (a + 1) * 128], scalar=SS[i][:, 2 + a:3 + a],
                                                                              in1=gaint[:, hp * 256 + a * 128:hp * 256 + (a + 1) * 128], op0=ALU.mult, op1=ALU.mult),
                               reads=[R_o, R_SS[i], R_cb], writes=[R_Y1[i]])
                        op("pool", lambda e, i=i, t=t: e.tensor_tensor(out=YH[i][:], in0=Y1[i][:], in1=SG[t % 5][:], op=ALU.mult), reads=[R_Y1[i], R_SG[t % 5]], writes=[R_YH[i]])

                    def s6(t):
                        i = t % 2
                        tb = bankbf(6)
                        for a in range(2):
                            op("pe", lambda e, i=i, a=a: e.transpose(out=tb[:, 512 + a * 128:512 + (a + 1) * 128], in_=YH[i][:, a * 128:(a + 1) * 128], identity=ident[:]), reads=[R_YH[i], R_c], writes=[R_TRy])
                        op("act", lambda e, t=t: e.copy(out=YT[wi][:, :, t * 128:(t + 1) * 128], in_=tb[:, 512:768].rearrange("p (a q) -> p a q", a=2)), reads=[R_TRy], writes=[R_YT[wi]])

                    stages = [s0, s1, s2, s3, s4, s5, s6]
                    if small and "NST" in small:
                        stages = stages[:small["NST"]]
                    for jj in range(NTB + len(stages) - 1):
                        lists = []
                        for k in reversed(range(1, len(stages))):
                            t = jj - k
                            if 0 <= t < NTB:
                                S.defer = []
                                stages[k](t)
                                lists.append(S.defer)
                                S.defer = None
                        while any(lists):
                            for L in lists:
                                if L:
                                    op(*L.pop(0))
                        if 0 <= jj < NTB:
                            stages[0](jj)
                        issue_casts(1)
                    for a in range(2):
                        op("sp", lambda e, a=a, hp=hp: e.dma_start(out=yhgT_d[2 * hp + a][:, 0:NTB * 128], in_=YT[wi][:, a, 0:NTB * 128]), reads=[R_YT[wi]], writes=[R_yhgd], dma=f"yhg{wi}")
                S.barrier()
        if stop_after == "B":
            return nc, "yhgT_d", S
        if "W" not in skip:
            issue_casts(len(castq))

        with ExitStack() as esc:
            def sbc(name, shape, dt):
                return esc.enter_context(nc.sbuf_tensor(name, list(shape), dt)).ap()

            wga = sbc("wga", [128, 8, D], BF16)
            wgh = sbc("wgh", [128, 8, D], BF16)
            wa_ = sbc("wa_", [128, 4, D], BF16)
            wh_ = sbc("wh_", [128, 8, D], BF16)
            wo_ = sbc("wo_", [128, 8, D], BF16)
            wr32 = sbc("wr32", [128, 8, NE], F32)
            ln1w = sbc("ln1w", [128, D], F32)
            ln1b = sbc("ln1b", [128, D], F32)
            rbt = sbc("rbt", [128, NE], F32)
            R_cc = Res()
            op("pool", lambda e: e.dma_start(out=wga[:], in_=w_in[:, GA0:GA0 + D].rearrange("(k p) j -> p k j", p=128)), writes=[R_cc], dma="pc0")
            op("pool", lambda e: e.dma_start(out=wgh[:], in_=w_in[:, GHG0:GHG0 + D].rearrange("(k p) j -> p k j", p=128)), writes=[R_cc], dma="pc1")
            op("pool", lambda e: e.dma_start(out=wa_[:], in_=w_a.rearrange("(k p) j -> p k j", p=128)), writes=[R_cc], dma="pc2")
            op("pool", lambda e: e.dma_start(out=wh_[:], in_=w_h.rearrange("(k p) j -> p k j", p=128)), writes=[R_cc], dma="pc3")
            op("pool", lambda e: e.dma_start(out=wo_[:], in_=w_o.rearrange("(k p) j -> p k j", p=128)), writes=[R_cc], dma="pc4")
            op("sp", lambda e: e.dma_start(out=wr32[:], in_=w_r.rearrange("(k p) j -> p k j", p=128)), writes=[R_cc], dma="c5")
            op("sp", lambda e: e.dma_start(out=ln1w[:], in_=v_ln1w[:, :]), writes=[R_cc], dma="c6")
            op("sp", lambda e: e.dma_start(out=ln1b[:], in_=v_ln1b[:, :]), writes=[R_cc], dma="c7")
            op("sp", lambda e: e.dma_start(out=rbt[:], in_=v_rb[:, :]), writes=[R_cc], dma="c8")
            S.barrier()

            def mk(name, shape, dt, n):
                return [sbc(f"{name}{i}", shape, dt) for i in range(n)], [Res() for _ in range(n)]

            xTs, R_xTs = mk("xTs", [128, 8, 512], BF16, 1)
            yas, R_yas = mk("yas", [128, 4, 512], BF16, 1)
            yhs, R_yhs = mk("yhs", [128, 8, 512], BF16, 1)
            xs, R_xs = mk("xs", [128, D], F32, 2)
            xTs, R_xTs, yas, R_yas, yhs, R_yhs = xTs * 2, R_xTs * 2, yas * 2, R_yas * 2, yhs * 2, R_yhs * 2
            mT, R_mT = mk("mT", [128, 8, 512], BF16, 1)
            EA, R_EA = mk("EA", [128, 1024], F32, 2)
            TM, R_TM = mk("TM", [128, 1024], F32, 2)
            Z, R_Z = mk("Z", [128, D], F32, 2)
            X1, R_X1 = mk("X1", [128, D], F32, 2)
            X1T32, R_X1T32 = mk("X1T32", [128, 8, 128], F32, 2)
            X1Tb, R_X1Tb = mk("X1Tb", [128, 8, 512], BF16, 1)
            X1Tb, R_X1Tb = X1Tb * 2, R_X1Tb * 2
            st6, R_st6 = mk("st6", [128, 12], F32, 2)
            mv, R_mv = mk("mv", [128, 4], F32, 2)
            RT, R_RT = mk("RT", [128, 6, NE], F32, 2)
            r8, R_r8 = mk("r8", [128, 10, 8], F32, 2)
            Gs, R_Gs = mk("Gs", [128, 4, NE], F32, 2)
            RB = PBanks()

            def load_slab(s):
                i = s % 2
                sl = slice(s * 512, (s + 1) * 512)
                op("sp", lambda e, i=i: e.dma_start(out=xTs[i][:], in_=xT_d[:, :, sl].rearrange("k p t -> p k t")), reads=[R_xTd], writes=[R_xTs[i]], dma=f"l0{i}")
                op("sp", lambda e, i=i: e.dma_start(out=yas[i][:], in_=yattT_d[:, :, sl].rearrange("k p t -> p k t")), reads=[R_yattd], writes=[R_yas[i]], dma=f"l1{i}")
                op("sp", lambda e, i=i: e.dma_start(out=yhs[i][:], in_=yhgT_d[:, :, sl].rearrange("k p t -> p k t")), reads=[R_yhgd], writes=[R_yhs[i]], dma=f"l2{i}")

            NSC = small["NSC"] if small and "NSC" in small else 8
            if "C" in skip:
                NSC = 0
            for s in range(NSC):
                i = s % 2
                load_slab(s)
                for dc in range(8):
                    b0 = 4 * (dc % 2)
                    cs = slice(dc * 128, (dc + 1) * 128)
                    for kc in range(8):
                        op("pe", lambda e, kc=kc, b0=b0, cs=cs: e.matmul(bank(b0), lhsT=wga[:, kc, cs], rhs=xTs[i][:, kc, :], start=(kc == 0), stop=(kc == 7)), reads=[R_cc, R_xTs[i]], writes=[RB[b0]])
                    for kc in range(8):
                        op("pe", lambda e, kc=kc, b0=b0, cs=cs: e.matmul(bank(b0 + 1), lhsT=wgh[:, kc, cs], rhs=xTs[i][:, kc, :], start=(kc == 0), stop=(kc == 7)), reads=[R_cc, R_xTs[i]], writes=[RB[b0 + 1]])
                    for kc in range(4):
                        op("pe", lambda e, kc=kc, b0=b0, cs=cs: e.matmul(bank(b0 + 2), lhsT=wa_[:, kc, cs], rhs=yas[i][:, kc, :], start=(kc == 0), stop=(kc == 3)), reads=[R_cc, R_yas[i]], writes=[RB[b0 + 2]])
                    for kc in range(8):
                        op("pe", lambda e, kc=kc, b0=b0, cs=cs: e.matmul(bank(b0 + 3), lhsT=wh_[:, kc, cs], rhs=yhs[i][:, kc, :], start=(kc == 0), stop=(kc == 7)), reads=[R_cc, R_yhs[i]], writes=[RB[b0 + 3]])
                    j = dc % 2
                    op("act", lambda e, b0=b0, j=j: e.activation(out=EA[j][:], in_=ps[:, b0 * 512:(b0 + 2) * 512], func=AF.Exp, scale=-1.0), reads=[RB[b0], RB[b0 + 1]], writes=[R_EA[j]])
                    op("act", lambda e, j=j: e.activation(out=EA[j][:], in_=EA[j][:], func=AF.Ln, bias=1.0, scale=1.0), reads=[R_EA[j]], writes=[R_EA[j]])
                    op("act", lambda e, j=j: e.activation(out=EA[j][:], in_=EA[j][:], func=AF.Exp, scale=-1.0), reads=[R_EA[j]], writes=[R_EA[j]])
                    op("dve", lambda e, b0=b0, j=j: e.tensor_tensor(out=TM[j][:], in0=EA[j][:], in1=ps[:, (b0 + 2) * 512:(b0 + 4) * 512], op=ALU.mult), reads=[R_EA[j], RB[b0 + 2], RB[b0 + 3]], writes=[R_TM[j]])
                    op("dve", lambda e, j=j, dc=dc: e.tensor_tensor(out=mT[0][:, dc, :], in0=TM[j][:, 0:512], in1=TM[j][:, 512:1024], op=ALU.add), reads=[R_TM[j]], writes=[R_mT[0]])
                def sub_chain(sub):
                        t = s * 4 + sub
                        j = t % 2
                        bo = 2 * (sub % 2)
                        op("sp", lambda e, j=j, t=t: e.dma_start(out=xs[j][:], in_=x_d[t * 128:(t + 1) * 128, :]), writes=[R_xs[j]], dma=f"l3{j}")
                        for half in range(2):
                            for kc in range(8):
                                op("pe", lambda e, kc=kc, half=half, bo=bo, sub=sub: e.matmul(bank(bo + half), lhsT=mT[0][:, kc, sub * 128:(sub + 1) * 128], rhs=wo_[:, kc, half * 512:(half + 1) * 512], start=(kc == 0), stop=(kc == 7)),
                                   reads=[R_mT[0], R_cc], writes=[RB[bo + half]])
                        op("dve", lambda e, j=j, bo=bo, sub=sub: e.scalar_tensor_tensor(out=Z[j][:], in0=xs[j][:], scalar=ALPHA, in1=ps[:, bo * 512:(bo + 2) * 512], op0=ALU.mult, op1=ALU.add),
                           reads=[R_xs[j], RB[bo], RB[bo + 1]], writes=[R_Z[j]])
                        emit_ln(op, Z[j], R_Z[j], X1[j], R_X1[j], st6[j], R_st6[j], mv[j], R_mv[j], ln1w, ln1b, R_cc, "dve")
                        op("sp", lambda e, j=j, t=t: e.dma_start(out=x1_d[t * 128:(t + 1) * 128, :], in_=X1[j][:]), reads=[R_X1[j]], writes=[R_x1d], dma=f"x1o{j}")
                        bt = 4 + 2 * (sub % 2)
                        for kc in range(8):
                            op("pe", lambda e, kc=kc, bt=bt, j=j: e.transpose(out=ps[:, bt * 512 + kc * 128:bt * 512 + (kc + 1) * 128], in_=X1[j][:, kc * 128:(kc + 1) * 128], identity=identf[:]),
                               reads=[R_X1[j], R_c], writes=[RB[bt], RB[bt + 1]])
                        op("act", lambda e, bt=bt, j=j: e.copy(out=X1T32[j][:], in_=ps[:, bt * 512:(bt + 2) * 512].rearrange("p (k q) -> p k q", k=8)), reads=[RB[bt], RB[bt + 1]], writes=[R_X1T32[j]])
                        op("act", lambda e, j=j, sub=sub: e.copy(out=X1Tb[i][:, :, sub * 128:(sub + 1) * 128], in_=X1T32[j][:]), reads=[R_X1T32[j]], writes=[R_X1Tb[i]])
                        for kc in range(8):
                            op("pe", lambda e, kc=kc, j=j, bt=bt: e.matmul(bank(bt)[:, 0:NE], lhsT=X1T32[j][:, kc, :], rhs=wr32[:, kc, :], start=(kc == 0), stop=(kc == 7)), reads=[R_X1T32[j], R_cc], writes=[RB[bt], RB[bt + 1]])
                        emit_route(op, bank(bt)[:, 0:NE], [RB[bt], RB[bt + 1]], RT[j], R_RT[j], r8[j], R_r8[j], rbt, R_cc, Gs[i][:, sub, :], R_Gs[i])

                for pair in ((0, 1), (2, 3)):
                    lists = []
                    for sub in pair:
                        S.defer = []
                        sub_chain(sub)
                        lists.append(S.defer)
                        S.defer = None
                    while any(lists):
                        for L in lists:
                            if L:
                                op(*L.pop(0))
                op("sp", lambda e, i=i, s=s: e.dma_start(out=x1T_d[:, :, s * 512:(s + 1) * 512].rearrange("k p t -> p k t"), in_=X1Tb[i][:]), reads=[R_X1Tb[i]], writes=[R_x1Td], dma=f"x1T{i}")
                op("sp", lambda e, i=i, s=s: e.dma_start(out=G_d[s * 512:(s + 1) * 512, :].rearrange("(a p) j -> p a j", p=128), in_=Gs[i][:]), reads=[R_Gs[i]], writes=[R_Gd], dma=f"G{i}")
            S.barrier()
        if stop_after == "C":
            return nc, "x1_d", S

        with ExitStack() as esd:
            def sbd(name, shape, dt):
                return esd.enter_context(nc.sbuf_tensor(name, list(shape), dt)).ap()

            wpg = sbd("wpg", [128, 8, D], BF16)
            wpp = sbd("wpp", [128, 2, D], BF16)
            ln2w = sbd("ln2w", [128, D], F32)
            ln2b = sbd("ln2b", [128, D], F32)
            R_cd = Res()
            op("pool", lambda e: e.dma_start(out=wpg[:], in_=w_pg.rearrange("(k p) j -> p k j", p=128)), writes=[R_cd], dma="pc0")
            op("pool", lambda e: e.dma_start(out=wpp[:], in_=w_pp.rearrange("(k p) j -> p k j", p=128)), writes=[R_cd], dma="pc1")
            op("sp", lambda e: e.dma_start(out=ln2w[:], in_=v_ln2w[:, :]), writes=[R_cd], dma="c2")
            op("sp", lambda e: e.dma_start(out=ln2b[:], in_=v_ln2b[:, :]), writes=[R_cd], dma="c3")
            S.barrier()

            def mk(name, shape, dt, n):
                return [sbd(f"{name}{i}", shape, dt) for i in range(n)], [Res() for _ in range(n)]

            NWB = 3
            WG, R_WG = mk("WG", [128, 8, 256], BF16, NWB)
            WU, R_WU = mk("WU", [128, 8, 256], BF16, NWB)
            WD, R_WD = mk("WD", [128, 2, D], BF16, NWB)
            hT, R_hT = mk("hT", [128, 8, 512], BF16, 2)
            x1s, R_x1s = mk("x1s", [128, 4, D], F32, 2)
            Gl, R_Gl = mk("Gl", [128, 4, NE], F32, 2)
            pbf, R_pbf = mk("pbf", [128, 4, 256], BF16, 2)
            pT, R_pT = mk("pT", [128, 2, 512], BF16, 1)
            yacc, R_yacc = mk("yacc", [128, 4, D], F32, 2)
            SGt, R_SGt = mk("SGt", [128, 512], F32, 2)
            ACTt, R_ACTt = mk("ACTt", [128, 512], BF16, 4)
            EP, R_EP = mk("EP", [128, D], F32, 2)
            OUT, R_OUT = mk("OUT", [128, D], F32, 2)
            st6, R_st6 = mk("st6d", [128, 12], F32, 2)
            mv, R_mv = mk("mvd", [128, 4], F32, 2)
            RB = PBanks()
            NEX = NE + 1
            NSD = small["NSD"] if small and "NSD" in small else 8
            seq = [(s, e_) for s in range(NSD) for e_ in range(NEX)]

            def load_w(n):
                s, e_ = seq[n]
                b = n % NWB
                op("sp", lambda e, b=b, e_=e_: e.dma_start(out=WG[b][:], in_=wg_d[e_].rearrange("p (k j) -> p k j", k=8)), reads=[R_wcast], writes=[R_WG[b]], dma=f"wg{b}")
                op("sp", lambda e, b=b, e_=e_: e.dma_start(out=WU[b][:], in_=wu_d[e_].rearrange("p (k j) -> p k j", k=8)), reads=[R_wcast], writes=[R_WU[b]], dma=f"wu{b}")
                op("sp", lambda e, b=b, e_=e_: e.dma_start(out=WD[b][:], in_=wd_d[e_].rearrange("p (k j) -> p k j", k=2)), reads=[R_wcast], writes=[R_WD[b]], dma=f"wd{b}")

            def load_slab(s):
                i = s % 2
                sl = slice(s * 512, (s + 1) * 512)
                op("sp", lambda e, i=i: e.dma_start(out=hT[i][:], in_=x1T_d[:, :, sl].rearrange("k p t -> p k t")), reads=[R_x1Td], writes=[R_hT[i]], dma=f"m0{i}")
                op("sp", lambda e, i=i: e.dma_start(out=x1s[i][:], in_=x1_d[sl, :].rearrange("(a p) j -> p a j", p=128)), reads=[R_x1d], writes=[R_x1s[i]], dma=f"m1{i}")
                op("sp", lambda e, i=i: e.dma_start(out=Gl[i][:], in_=G_d[sl, :].rearrange("(a p) j -> p a j", p=128)), reads=[R_Gd], writes=[R_Gl[i]], dma=f"m2{i}")
                op("pool", lambda e, i=i: e.dma_start(out=pbf[i][:], in_=p_d[sl, :].rearrange("(a p) j -> p a j", p=128)), writes=[R_pbf[i]], dma=f"m3{i}")

            load_slab(0)
            load_w(0)
            load_w(1)

            def gu(n, c):
                s, e_ = seq[n]
                b = n % NWB
                i = s % 2
                for kc in range(8):
                    op("pe", lambda e, kc=kc, b=b, c=c, i=i: e.matmul(bank(2 * c), lhsT=WG[b][:, kc, c * 128:(c + 1) * 128], rhs=hT[i][:, kc, :], start=(kc == 0), stop=(kc == 7)), reads=[R_WG[b], R_hT[i]], writes=[RB[2 * c]])
                for kc in range(8):
                    op("pe", lambda e, kc=kc, b=b, c=c, i=i: e.matmul(bank(2 * c + 1), lhsT=WU[b][:, kc, c * 128:(c + 1) * 128], rhs=hT[i][:, kc, :], start=(kc == 0), stop=(kc == 7)), reads=[R_WU[b], R_hT[i]], writes=[RB[2 * c + 1]])
                a_ = (n % 2) * 2 + c
                op("act", lambda e, c=c: e.activation(out=SGt[c][:], in_=bank(2 * c), func=AF.Silu), reads=[RB[2 * c]], writes=[R_SGt[c]])
                op("dve", lambda e, c=c, a_=a_: e.tensor_tensor(out=ACTt[a_][:], in0=SGt[c][:], in1=bank(2 * c + 1), op=ALU.mult), reads=[R_SGt[c], RB[2 * c + 1]], writes=[R_ACTt[a_]])

            def down(n):
                s, e_ = seq[n]
                b = n % NWB
                i = s % 2
                for sub in range(4):
                    bo = 4 + 2 * (sub % 2)
                    for half in range(2):
                        for c in range(2):
                            a_ = (n % 2) * 2 + c
                            op("pe", lambda e, c=c, a_=a_, half=half, bo=bo, sub=sub, b=b: e.matmul(bank(bo + half), lhsT=ACTt[a_][:, sub * 128:(sub + 1) * 128], rhs=WD[b][:, c, half * 512:(half + 1) * 512], start=(c == 0), stop=(c == 1)),
                               reads=[R_ACTt[a_], R_WD[b]], writes=[RB[bo + half]])
                    if e_ < NE:
                        op("dve", lambda e, sub=sub, bo=bo, e_=e_, i=i: e.scalar_tensor_tensor(out=yacc[i][:, sub, :], in0=ps[:, bo * 512:(bo + 2) * 512], scalar=Gl[i][:, sub, e_:e_ + 1], in1=yacc[i][:, sub, :], op0=ALU.mult, op1=ALU.add),
                           reads=[RB[bo], RB[bo + 1], R_Gl[i], R_yacc[i]], writes=[R_yacc[i]])
                    else:
                        op("dve", lambda e, sub=sub, bo=bo, i=i: e.tensor_tensor(out=yacc[i][:, sub, :], in0=ps[:, bo * 512:(bo + 2) * 512], in1=yacc[i][:, sub, :], op=ALU.add),
                           reads=[RB[bo], RB[bo + 1], R_yacc[i]], writes=[R_yacc[i]])

            def slab_prologue(s):
                i = s % 2
                tb = bankbf(6)
                for a in range(4):
                    for k in range(2):
                        op("pe", lambda e, a=a, k=k, i=i: e.transpose(out=tb[:, k * 512 + a * 128:k * 512 + (a + 1) * 128], in_=pbf[i][:, a, k * 128:(k + 1) * 128], identity=ident[:]), reads=[R_pbf[i], R_c], writes=[RB[6]])
                op("act", lambda e: e.copy(out=pT[0][:], in_=tb[:, 0:1024].rearrange("p (k t) -> p k t", k=2)), reads=[RB[6]], writes=[R_pT[0]])
                for sub in range(4):
                    op("act", lambda e, sub=sub, i=i: e.mul(out=yacc[i][:, sub, :], in_=x1s[i][:, sub, :], mul=ALPHA), reads=[R_x1s[i]], writes=[R_yacc[i]])
                for sub in range(4):
                    gb = 4 if sub % 2 == 0 else 0
                    ep = EP[sub % 2]
                    R_ep = R_EP[sub % 2]
                    for half in range(2):
                        for kc in range(8):
                            op("pe", lambda e, kc=kc, half=half, sub=sub, i=i, gb=gb: e.matmul(bank(gb + half), lhsT=hT[i][:, kc, sub * 128:(sub + 1) * 128], rhs=wpg[:, kc, half * 512:(half + 1) * 512], start=(kc == 0), stop=(kc == 7)), reads=[R_hT[i], R_cd], writes=[RB[gb + half]])
                        for kc in range(2):
                            op("pe", lambda e, kc=kc, half=half, sub=sub, gb=gb: e.matmul(bank(gb + 2 + half), lhsT=pT[0][:, kc, sub * 128:(sub + 1) * 128], rhs=wpp[:, kc, half * 512:(half + 1) * 512], start=(kc == 0), stop=(kc == 1)), reads=[R_pT[0], R_cd], writes=[RB[gb + 2 + half]])
                    op("act", lambda e, gb=gb, ep=ep: e.activation(out=ep[:], in_=ps[:, gb * 512:(gb + 2) * 512], func=AF.Exp, scale=-1.0), reads=[RB[gb], RB[gb + 1]], writes=[R_ep])
                    op("act", lambda e, ep=ep: e.activation(out=ep[:], in_=ep[:], func=AF.Ln, bias=1.0, scale=1.0), reads=[R_ep], writes=[R_ep])
                    op("act", lambda e, ep=ep: e.activation(out=ep[:], in_=ep[:], func=AF.Exp, scale=-1.0), reads=[R_ep], writes=[R_ep])
                    op("dve", lambda e, gb=gb, ep=ep: e.tensor_tensor(out=ep[:], in0=ep[:], in1=ps[:, (gb + 2) * 512:(gb + 4) * 512], op=ALU.mult), reads=[R_ep, RB[gb + 2], RB[gb + 3]], writes=[R_ep])
                    op("dve", lambda e, sub=sub, ep=ep, i=i: e.tensor_tensor(out=yacc[i][:, sub, :], in0=yacc[i][:, sub, :], in1=ep[:], op=ALU.add), reads=[R_ep, R_yacc[i]], writes=[R_yacc[i]])

            def slab_epilogue_sub(s, sub):
                t = s * 4 + sub
                j = t % 2
                i = s % 2
                emit_ln(op, yacc[i][:, sub, :], R_yacc[i], OUT[j], R_OUT[j], st6[j], R_st6[j], mv[j], R_mv[j], ln2w, ln2b, R_cd, "dve", in_readonly=True)
                op("sp", lambda e, j=j, t=t: e.dma_start(out=out_d[t * 128:(t + 1) * 128, :], in_=OUT[j][:]), reads=[R_OUT[j]], dma=f"out{j}")

            N = len(seq)
            for n in range(N):
                s, e_ = seq[n]
                if e_ == 0:
                    if s + 1 < NSD:
                        load_slab(s + 1)
                    slab_prologue(s)
                    gu(n, 0)
                    gu(n, 1)
                if n + 2 < N:
                    load_w(n + 2)
                nxt_same_slab = (n + 1 < N) and seq[n + 1][0] == s
                if nxt_same_slab:
                    gu(n + 1, 0)
                down(n)
                if nxt_same_slab:
                    gu(n + 1, 1)
                if s >= 1 and 2 <= e_ < 6:
                    slab_epilogue_sub(s - 1, e_ - 2)
                if n == N - 1:
                    for sub in range(4):
                        slab_epilogue_sub(s, sub)
            S.barrier()
    return nc, "out", S


def emit_ln(op, zin, R_zin, xout, R_xout, st6, R_st6, mv, R_mv, lw, lb, R_const, eng, in_readonly=False):
    for hf in range(2):
        op("dve", lambda e, hf=hf: e.bn_stats(out=st6[:, hf * 6:(hf + 1) * 6], in_=zin[:, hf * 512:(hf + 1) * 512]), reads=[R_zin], writes=[R_st6])
    op("dve", lambda e: e.bn_aggr(out=mv[:, 0:2], in_=st6[:]), reads=[R_st6], writes=[R_mv])
    op("act", lambda e: e.activation(out=mv[:, 2:3], in_=mv[:, 1:2], func=AF.Ln, bias=LN_EPS, scale=1.0), reads=[R_mv], writes=[R_mv])
    op("act", lambda e: e.activation(out=mv[:, 2:3], in_=mv[:, 2:3], func=AF.Exp, scale=-0.5), reads=[R_mv], writes=[R_mv])
    op("dve", lambda e: e.tensor_scalar(out=xout[:], in0=zin, scalar1=mv[:, 0:1], scalar2=mv[:, 2:3], op0=ALU.subtract, op1=ALU.mult), reads=[R_zin, R_mv], writes=[R_xout])
    op("dve", lambda e: e.tensor_tensor(out=xout[:], in0=xout[:], in1=lw[:], op=ALU.mult), reads=[R_xout, R_const], writes=[R_xout])
    op("dve", lambda e: e.tensor_tensor(out=xout[:], in0=xout[:], in1=lb[:], op=ALU.add), reads=[R_xout, R_const], writes=[R_xout])


def emit_route(op, logits, R_logits, RT, R_RT, r8, R_r8, rbt, R_const, Gout, R_Gout):
    BIG = 1.0e4
    s_ = RT[:, 0, :]
    sel = RT[:, 1, :]
    tmp = RT[:, 2, :]
    selm = RT[:, 3, :]
    em = RT[:, 4, :]
    g_ = RT[:, 5, :]
    op("act", lambda e: e.activation(out=s_, in_=logits, func=AF.Exp, scale=-1.0), reads=R_logits, writes=[R_RT])
    op("act", lambda e: e.activation(out=s_, in_=s_, func=AF.Ln, bias=1.0, scale=1.0), reads=[R_RT], writes=[R_RT])
    op("act", lambda e: e.activation(out=s_, in_=s_, func=AF.Exp, scale=-1.0), reads=[R_RT], writes=[R_RT])
    op("dve", lambda e: e.tensor_tensor(out=sel, in0=s_, in1=rbt[:], op=ALU.add), reads=[R_RT, R_const], writes=[R_RT])
    for gidx in range(8):
        op("dve", lambda e, gidx=gidx: e.max(out=r8[:, gidx, :], in_=sel[:, gidx * 8:(gidx + 1) * 8]), reads=[R_RT], writes=[R_r8])
    op("dve", lambda e: e.tensor_tensor(out=r8[:, 8, :], in0=r8[:, 0:8, 0], in1=r8[:, 0:8, 1], op=ALU.add), reads=[R_r8], writes=[R_r8])
    op("dve", lambda e: e.max(out=r8[:, 9, :], in_=r8[:, 8, :]), reads=[R_r8], writes=[R_r8])
    op("dve", lambda e: e.tensor_scalar(out=r8[:, 8, :], in0=r8[:, 8, :], scalar1=r8[:, 9, 3:4], scalar2=None, op0=ALU.is_ge), reads=[R_r8], writes=[R_r8])
    op("dve", lambda e: e.tensor_tensor(out=selm.rearrange("p (g j) -> p g j", g=8), in0=sel.rearrange("p (g j) -> p g j", g=8), in1=r8[:, 8, :].unsqueeze(2).to_broadcast([128, 8, 8]), op=ALU.mult), reads=[R_RT, R_r8], writes=[R_RT])
    op("dve", lambda e: e.tensor_scalar(out=r8[:, 8, :], in0=r8[:, 8, :], scalar1=-1.0, scalar2=BIG, op0=ALU.add, op1=ALU.mult), reads=[R_r8], writes=[R_r8])
    op("dve", lambda e: e.tensor_tensor(out=selm.rearrange("p (g j) -> p g j", g=8), in0=selm.rearrange("p (g j) -> p g j", g=8), in1=r8[:, 8, :].unsqueeze(2).to_broadcast([128, 8, 8]), op=ALU.add), reads=[R_RT, R_r8], writes=[R_RT])
    op("dve", lambda e: e.max(out=r8[:, 9, :], in_=selm), reads=[R_RT], writes=[R_r8])
    op("dve", lambda e: e.tensor_scalar(out=em, in0=selm, scalar1=r8[:, 9, 7:8], scalar2=None, op0=ALU.is_ge), reads=[R_RT, R_r8], writes=[R_RT])
    op("dve", lambda e: e.tensor_tensor(out=g_, in0=s_, in1=em, op=ALU.mult), reads=[R_RT], writes=[R_RT])
    op("dve", lambda e: e.tensor_reduce(out=r8[:, 9, 0:1], in_=g_, axis=mybir.AxisListType.X, op=ALU.add), reads=[R_RT, R_r8], writes=[R_r8])
    op("dve", lambda e: e.reciprocal(out=r8[:, 9, 1:2], in_=r8[:, 9, 0:1]), reads=[R_r8], writes=[R_r8])
    op("dve", lambda e: e.tensor_scalar(out=Gout, in0=g_, scalar1=r8[:, 9, 1:2], scalar2=2.5, op0=ALU.mult, op1=ALU.mult), reads=[R_RT, R_r8], writes=[R_Gout])


_PROG = {}


def _in_maps(inputs):
    c = make_consts()
    f = lambda a: np.ascontiguousarray(np.asarray(a, dtype=np.float32))
    bc = lambda v: np.ascontiguousarray(np.broadcast_to(np.asarray(v, np.float32)[None, :], (128, v.shape[-1])))
    shared = {
        "w_in": f(inputs["w_in"][0]), "w_a": f(inputs["w_branch_att"][0]), "w_h": f(inputs["w_branch_hgrn"][0]),
        "w_o": f(inputs["w_out"][0]), "w_r": f(inputs["router_w"][0]),
        "ewg": f(inputs["expert_w_gate"][0]), "ewu": f(inputs["expert_w_up"][0]), "ewd": f(inputs["expert_w_down"][0]),
        "swg": f(inputs["shared_w_gate"][0]), "swu": f(inputs["shared_w_up"][0]), "swd": f(inputs["shared_w_down"][0]),
        "w_pg": f(inputs["ple_gate_w"][0]), "w_pp": f(inputs["ple_proj_w"][0]),
        "v_lb0": bc(np.asarray(inputs["hgrn_lb_logits"])[0]), "v_lb1": bc(np.asarray(inputs["hgrn_lb_logits"])[1]),
        "v_gain": bc(np.asarray(inputs["hgrn_norm_w"])[0]),
        "v_ln1w": bc(np.asarray(inputs["ln1_w"])[0]), "v_ln1b": bc(np.asarray(inputs["ln1_b"])[0]),
        "v_ln2w": bc(np.asarray(inputs["ln2_w"])[0]), "v_ln2b": bc(np.asarray(inputs["ln2_b"])[0]),
        "v_rb": bc(np.asarray(inputs["router_bias"])[0]),
    }
    shared.update(c)
    x = np.asarray(inputs["x"], np.float32)
    p = np.asarray(inputs["p"], np.float32)[0]
    maps = []
    for b in range(x.shape[0]):
        m = dict(shared)
        m["x"] = np.ascontiguousarray(x[b])
        m["p"] = np.ascontiguousarray(p[b])
        maps.append(m)
    return maps


def kernel(**inputs):
    if "nc" not in _PROG:
        _PROG["nc"] = build_program("D")[0]
    nc = _PROG["nc"]
    maps = _in_maps(inputs)
    res = run_bass_kernel_spmd(nc, maps, core_ids=list(range(len(maps))))
    return np.stack([np.asarray(r["out"], dtype=np.float32) for r in res.results], axis=0)
```

```python
import numpy as np
import ml_dtypes
from contextlib import ExitStack
import concourse.bass as bass
import concourse.mybir as mybir
from concourse.bass_utils import run_bass_kernel_spmd

F32 = mybir.dt.float32
BF16 = mybir.dt.bfloat16
AF = mybir.ActivationFunctionType
ALU = mybir.AluOpType

T = 4096
D = 1024
NT = 32
NIN = 10752
QA0, KA0, VA0, QH0, FH0, IH0, GH0, GA0, GHG0 = 0, 1536, 3072, 4608, 5632, 6656, 7680, 8704, 9728
DILS = (1, 4, 16)
ALPHA = float(2.0 ** 0.25)
LN_EPS = 1e-5
RMS_EPS = 1e-6
NE = 64
NEG = -30000.0


class Res:
    __slots__ = ("name", "w", "rs", "excl")

    def __init__(self, name="", excl=False):
        self.name = name
        self.w = None
        self.rs = {}
        self.excl = excl


def PBanks():
    return [Res("bank%d" % i, excl=True) for i in range(8)]


class Sched:
    def __init__(self, nc, plan=None):
        self.nc = nc
        self.engs = {"pe": nc.tensor, "act": nc.scalar, "dve": nc.vector, "pool": nc.gpsimd, "sp": nc.sync}
        self.sems = {}
        self.count = {}
        self.known = {k: {} for k in self.engs}
        self.nins = 0
        self.plan = plan
        self.needed = {}
        self.defer = None

    def sem(self, name):
        if name not in self.sems:
            self.sems[name] = self.nc.alloc_semaphore(name="s_" + name)
            self.count[name] = 0
        return self.sems[name]

    def _wait(self, eng, s, v):
        self.needed.setdefault(s, set()).add(v)
        if self.plan is not None and s.startswith("e_"):
            v = self.plan[s][v]
        self.engs[eng].wait_ge(self.sem(s), v)

    def op(self, eng, fn, reads=(), writes=(), dma=None):
        if self.defer is not None:
            self.defer.append((eng, fn, tuple(reads), tuple(writes), dma))
            return None
        ex = [r for r in reads if r.excl and r not in writes]
        if ex:
            writes = tuple(writes) + tuple(ex)
        deps = {}
        own = "e_" + eng

        def add(tok):
            if tok is None:
                return
            s, v = tok
            if deps.get(s, 0) < v:
                deps[s] = v

        for r in reads:
            add(r.w)
        for w in writes:
            add(w.w)
            for s, v in w.rs.items():
                add((s, v))
        e = self.engs[eng]
        kn = self.known[eng]
        for s, v in deps.items():
            if eng == "pe" and s == own:
                continue
            if kn.get(s, 0) < v:
                self._wait(eng, s, v)
                kn[s] = v
        if dma is None:
            s = own
            inc = 1
        else:
            s = "d_" + dma
            inc = 16
        h = self.sem(s)
        self.count[s] += inc
        tok = (s, self.count[s])
        ins = fn(e)
        if dma is not None or self.plan is None or tok[1] in self.plan.get(s, {}):
            ins.then_inc(h, inc)
        self.nins += 1
        for r in reads:
            if r.rs.get(s, 0) < tok[1]:
                r.rs[s] = tok[1]
        for w in writes:
            w.w = tok
            w.rs = {}
        return tok

    def barrier(self):
        for eng in self.engs:
            kn = self.known[eng]
            for s, v in self.count.items():
                if v > 0 and kn.get(s, 0) < v:
                    self._wait(eng, s, v)
                    kn[s] = v

    def make_plan(self):
        return {s: {v: i + 1 for i, v in enumerate(sorted(vs))} for s, vs in self.needed.items()}


def make_consts():
    bf = ml_dtypes.bfloat16
    c = {}
    c["c_ident_bf"] = np.eye(128, dtype=np.float32).astype(bf)
    c["c_ident_f"] = np.eye(128, dtype=np.float32)
    k = np.arange(128)[:, None].astype(np.float64)
    q = np.arange(128)[None, :].astype(np.float64)
    ab = np.zeros((128, 48, 128), np.float32)
    for g, d in enumerate(DILS):
        for h in range(8):
            slope = 2.0 ** (-(h + 1))
            cur = np.where(k <= q, -slope * d * (q - k), NEG)
            prev = np.where(k >= q, -slope * d * (q + 128 - k), NEG)
            ab[:, (g * 8 + h) * 2 + 0, :] = cur
            ab[:, (g * 8 + h) * 2 + 1, :] = prev
    c["c_abias"] = ab.reshape(128, 48 * 128).astype(bf)
    c["c_ones_bf"] = np.ones((128, 64), np.float32).astype(bf)
    s = np.arange(128)[:, None]
    t = np.arange(128)[None, :]
    same = (s // 64) == (t // 64)
    c["c_tri_incl"] = (same & (s <= t)).astype(np.float32)
    c["c_tri_after"] = (same & (s > t)).astype(np.float32)
    sel = np.zeros((128, 2), np.float32)
    sel[63, 0] = 1.0
    sel[127, 1] = 1.0
    c["c_sel2"] = sel
    cm = np.zeros((128, 2), np.float32)
    cm[:64, 0] = 1.0
    cm[64:, 1] = 1.0
    c["c_cm"] = cm
    return c


def build_program(stop_after="D", dbg=(), skip=(), small=None, inject=()):
    _, _, S1 = _build(stop_after, dbg, skip, small, None, inject)
    nc, name, S2 = _build(stop_after, dbg, skip, small, S1.make_plan(), inject)
    return nc, name


def _build(stop_after, dbg, skip, small, plan, inject=()):
    nc = bass.Bass("TRN2", target_bir_lowering=False)

    def din(name, shape, dt=F32):
        return nc.dram_tensor(name, list(shape), dt, kind="ExternalInput").ap()

    def dscr(name, shape, dt):
        if name in inject:
            return nc.dram_tensor(name, list(shape), dt, kind="ExternalInput").ap()
        if name in dbg:
            return nc.dram_tensor(name, list(shape), dt, kind="ExternalOutput").ap()
        return nc.dram_tensor(name, list(shape), dt).ap()

    x_d = din("x", [T, D])
    p_d = din("p", [T, 256])
    w_in = din("w_in", [D, NIN])
    w_a = din("w_a", [512, D])
    w_h = din("w_h", [D, D])
    w_o = din("w_o", [D, D])
    w_r = din("w_r", [D, NE])
    ewg = din("ewg", [NE, D, 256])
    ewu = din("ewu", [NE, D, 256])
    ewd = din("ewd", [NE, 256, D])
    swg = din("swg", [D, 256])
    swu = din("swu", [D, 256])
    swd = din("swd", [256, D])
    w_pg = din("w_pg", [D, D])
    w_pp = din("w_pp", [256, D])
    v_lb0 = din("v_lb0", [128, D])
    v_lb1 = din("v_lb1", [128, D])
    v_gain = din("v_gain", [128, D])
    v_ln1w = din("v_ln1w", [128, D])
    v_ln1b = din("v_ln1b", [128, D])
    v_ln2w = din("v_ln2w", [128, D])
    v_ln2b = din("v_ln2b", [128, D])
    v_rb = din("v_rb", [128, NE])
    c_ident_bf = din("c_ident_bf", [128, 128], BF16)
    c_ident_f = din("c_ident_f", [128, 128])
    c_abias = din("c_abias", [128, 48 * 128], BF16)
    c_ones_bf = din("c_ones_bf", [128, 64], BF16)
    c_tri_incl = din("c_tri_incl", [128, 128])
    c_tri_after = din("c_tri_after", [128, 128])
    c_sel2 = din("c_sel2", [128, 2])
    c_cm = din("c_cm", [128, 2])
    out_d = nc.dram_tensor("out", [T, D], F32, kind="ExternalOutput").ap()

    xT_d = dscr("xT_d", [8, 128, T], BF16)
    yattT_d = dscr("yattT_d", [4, 128, T], BF16)
    yhgT_d = dscr("yhgT_d", [8, 128, T], BF16)
    x1T_d = dscr("x1T_d", [8, 128, T], BF16)
    x1_d = dscr("x1_d", [T, D], F32)
    G_d = dscr("G_d", [T, NE], F32)
    wg_d = dscr("wg_d", [NE + 1, 128, 8 * 256], BF16)
    wu_d = dscr("wu_d", [NE + 1, 128, 8 * 256], BF16)
    wd_d = dscr("wd_d", [NE + 1, 128, 2 * 1024], BF16)

    S = Sched(nc, plan)
    op = S.op
    ps = nc.alloc_psum_tensor("ps", [128, 4096], F32).ap()

    def bank(b):
        return ps[:, b * 512:(b + 1) * 512]

    def bankbf(b):
        return bank(b).bitcast(BF16)

    R_xTd, R_yattd, R_yhgd, R_x1Td, R_x1d, R_Gd, R_wcast = [Res(n) for n in "xTd yattd yhgd x1Td x1d Gd wcast".split()]

    with ExitStack() as esg:
        def sbg(name, shape, dt):
            return esg.enter_context(nc.sbuf_tensor(name, list(shape), dt)).ap()

        ident = sbg("ident", [128, 128], BF16)
        identf = sbg("identf", [128, 128], F32)
        R_c = Res("consts")
        op("sp", lambda e: e.dma_start(out=ident[:], in_=c_ident_bf[:, :]), writes=[R_c], dma="c0")
        op("sp", lambda e: e.dma_start(out=identf[:], in_=c_ident_f[:, :]), writes=[R_c], dma="c1")
        S.barrier()

        castq = []
        for e_ in range(NE + 1):
            sg_, su_, sd_ = (ewg[e_], ewu[e_], ewd[e_]) if e_ < NE else (swg, swu, swd)
            castq.append((wg_d[e_].rearrange("p (k j) -> p k j", k=8), sg_.rearrange("(k p) j -> p k j", p=128)))
            castq.append((wu_d[e_].rearrange("p (k j) -> p k j", k=8), su_.rearrange("(k p) j -> p k j", p=128)))
            castq.append((wd_d[e_].rearrange("p (k j) -> p k j", k=2), sd_.rearrange("(k p) j -> p k j", p=128)))

        def issue_casts(n):
            for _ in range(n):
                if castq:
                    o_, i_ = castq.pop(0)
                    op("pool", lambda e, o_=o_, i_=i_: e.dma_start(out=o_, in_=i_), writes=[R_wcast], dma="wcast")

        with ExitStack() as esx:
            xT = esx.enter_context(nc.sbuf_tensor("xT", [128, 8, T], BF16)).ap()
            R_xT = Res("xT")
            with ExitStack() as es0:
                xb = [es0.enter_context(nc.sbuf_tensor(f"xb{i}", [128, D], BF16)).ap() for i in range(2)]
                R_xb = [Res(), Res()]
                R_pb = [Res(excl=True), Res(excl=True)]
                for t in range(0 if "0" in skip else NT):
                    i = t % 2
                    op("pool", lambda e, t=t, i=i: e.dma_start(out=xb[i][:], in_=x_d[t * 128:(t + 1) * 128, :]),
                       writes=[R_xb[i]], dma=f"xb{i}")
                    pb = bankbf(i)
                    for k in range(8):
                        op("pe", lambda e, k=k, i=i, pb=pb: e.transpose(out=pb[:, k * 128:(k + 1) * 128], in_=xb[i][:, k * 128:(k + 1) * 128], identity=ident[:]),
                           reads=[R_xb[i], R_c], writes=[R_pb[i]])
                    eng = "act" if i else "dve"
                    if eng == "act":
                        op("act", lambda e, t=t, pb=pb: e.copy(out=xT[:, :, t * 128:(t + 1) * 128], in_=pb.rearrange("p (k j) -> p k j", k=8)),
                           reads=[R_pb[i]], writes=[R_xT])
                    else:
                        op("dve", lambda e, t=t, pb=pb: e.tensor_copy(out=xT[:, :, t * 128:(t + 1) * 128], in_=pb.rearrange("p (k j) -> p k j", k=8)),
                           reads=[R_pb[i]], writes=[R_xT])
                for k in range(0 if "0" in skip else 8):
                    op("sp", lambda e, k=k: e.dma_start(out=xT_d[k], in_=xT[:, k, :]), reads=[R_xT], writes=[R_xTd], dma="xTd")
                S.barrier()

            with ExitStack() as esa:
                def sba(name, shape, dt):
                    return esa.enter_context(nc.sbuf_tensor(name, list(shape), dt)).ap()

                abias = sba("abias", [128, 48 * 128], BF16)
                ones_bf = sba("ones_bf", [128, 64], BF16)
                R_ca = Res()
                op("sp", lambda e: e.dma_start(out=abias[:], in_=c_abias[:, :]), writes=[R_ca], dma="c0")
                op("sp", lambda e: e.dma_start(out=ones_bf[:], in_=c_ones_bf[:, :]), writes=[R_ca], dma="c1")
                wq = [sba(f"wq{i}", [128, 8, 128], BF16) for i in range(2)]
                wk = [sba(f"wk{i}", [128, 8, 128], BF16) for i in range(2)]
                wv = [sba(f"wv{i}", [128, 8, 128], BF16) for i in range(2)]
                R_w = [Res(), Res()]
                QT = sba("QT", [128, T], BF16)
                KTm = [sba("KTm0", [128, T], BF16), sba("KTm1", [128, T], BF16)]
                Vb = sba("Vb", [128, 32, 128], BF16)
                R_QT, R_KT, R_Vb = Res(), Res(), Res()
                accN = sba("accN", [128, T], F32)
                accD = sba("accD", [128, T], F32)
                R_acc = Res()
                ybuf = sba("ybuf", [128, T], BF16)
                R_yb = Res()
                PT = [sba(f"PT{i}", [128, 512], BF16) for i in range(4)]
                R_PT = [Res() for _ in range(4)]
                RB = PBanks()
                op("pool", lambda e: e.memset(KTm[0][64:128, :], 0.0), writes=[R_KT])
                op("pool", lambda e: e.memset(KTm[1][0:64, :], 0.0), writes=[R_KT])
                S.barrier()

                its = [(hp, g) for hp in range(4) for g in range(3)]
                if "A" in skip:
                    its = []
                if small and "AITS" in small:
                    its = its[:small["AITS"]]

                def load_w(idx):
                    hp, g = its[idx]
                    i = idx % 2
                    col = g * 512 + hp * 128
                    for wt, base in ((wq[i], QA0), (wk[i], KA0), (wv[i], VA0)):
                        op("pool", lambda e, wt=wt, c0=base + col: e.dma_start(out=wt[:], in_=w_in[:, c0:c0 + 128].rearrange("(k p) j -> p k j", p=128)),
                           writes=[R_w[i]], dma=f"aw{i}")

                if its:
                    load_w(0)
                for idx, (hp, g) in enumerate(its):
                    i = idx % 2
                    d = DILS[g]
                    if idx + 1 < len(its):
                        load_w(idx + 1)

                    def cols(blk, d=d):
                        n, r = blk // d, blk % d
                        return slice(n * 128 * d + r, (n + 1) * 128 * d, d)

                    for s in range(8):
                        bq, bk = 6, 7
                        for kc in range(8):
                            op("pe", lambda e, kc=kc, s=s, i=i: e.matmul(bank(6), lhsT=wq[i][:, kc, :], rhs=xT[:, kc, s * 512:(s + 1) * 512], start=(kc == 0), stop=(kc == 7)),
                               reads=[R_w[i], R_xT], writes=[RB[6]])
                        op("act", lambda e, s=s: e.mul(out=QT[:, s * 512:(s + 1) * 512], in_=bank(6), mul=0.125),
                           reads=[RB[6]], writes=[R_QT])
                        for kc in range(8):
                            op("pe", lambda e, kc=kc, s=s, i=i: e.matmul(bank(7), lhsT=wk[i][:, kc, :], rhs=xT[:, kc, s * 512:(s + 1) * 512], start=(kc == 0), stop=(kc == 7)),
                               reads=[R_w[i], R_xT], writes=[RB[7]])
                        op("dve", lambda e, s=s: e.tensor_copy(out=KTm[0][0:64, s * 512:(s + 1) * 512], in_=bank(7)[0:64, :]),
                           reads=[RB[7]], writes=[R_KT])
                        op("dve", lambda e, s=s: e.tensor_copy(out=KTm[1][64:128, s * 512:(s + 1) * 512], in_=bank(7)[64:128, :]),
                           reads=[RB[7]], writes=[R_KT])
                    for j in range(8):
                        b_ = 6 + (j % 2)
                        for bb in range(4):
                            blk = 4 * j + bb
                            for kc in range(8):
                                op("pe", lambda e, kc=kc, bb=bb, blk=blk, b_=b_, i=i: e.matmul(bank(b_)[:, bb * 128:(bb + 1) * 128], lhsT=xT[:, kc, cols(blk)], rhs=wv[i][:, kc, :], start=(kc == 0), stop=(kc == 7)),
                                   reads=[R_w[i], R_xT], writes=[RB[b_]])
                        if j % 2:
                            op("act", lambda e, j=j, b_=b_: e.copy(out=Vb[:, 4 * j:4 * j + 4, :], in_=bank(b_).rearrange("p (a q) -> p a q", a=4)),
                               reads=[RB[b_]], writes=[R_Vb])
                        else:
                            op("dve", lambda e, j=j, b_=b_: e.tensor_copy(out=Vb[:, 4 * j:4 * j + 4, :], in_=bank(b_).rearrange("p (a q) -> p a q", a=4)),
                               reads=[RB[b_]], writes=[R_Vb])

                    def stage1(blk):
                        hasprev = blk >= d
                        sb_ = blk % 4
                        bi = ((g * 8 + hp * 2) * 2) * 128
                        if hasprev:
                            op("pe", lambda e, bi=bi: e.matmul(bank(sb_), lhsT=ident[:], rhs=abias[:, bi:bi + 512], start=True, stop=False),
                               reads=[R_ca, R_c], writes=[RB[sb_]])
                        for a in range(2):
                            for c in range(2 if hasprev else 1):
                                kblk = blk - c * d
                                o_ = bank(sb_)[:, (a * 2 + c) * 128:(a * 2 + c + 1) * 128]
                                if not hasprev:
                                    bi2 = bi + (a * 2 + c) * 128
                                    op("pe", lambda e, o_=o_, bi2=bi2: e.matmul(o_, lhsT=ident[:], rhs=abias[:, bi2:bi2 + 128], start=True, stop=False),
                                       reads=[R_ca, R_c], writes=[RB[sb_]])
                                op("pe", lambda e, o_=o_, a=a, kblk=kblk, blk=blk: e.matmul(o_, lhsT=KTm[a][:, cols(kblk)], rhs=QT[:, cols(blk)], start=False, stop=((not hasprev) or (a == 1 and c == 1))),
                                   reads=[R_KT, R_QT], writes=[RB[sb_]])
                        if hasprev:
                            op("act", lambda e, sb_=sb_: e.activation(out=PT[sb_][:], in_=bank(sb_), func=AF.Exp),
                               reads=[RB[sb_]], writes=[R_PT[sb_]])
                        else:
                            op("act", lambda e, sb_=sb_: e.activation(out=PT[sb_].rearrange("p (a c q) -> p a c q", a=2, c=2)[:, :, 0, :],
                                                                      in_=bank(sb_).rearrange("p (a c q) -> p a c q", a=2, c=2)[:, :, 0, :], func=AF.Exp),
                               reads=[RB[sb_]], writes=[R_PT[sb_]])

                    def stage2(blk):
                        hasprev = blk >= d
                        sb_ = blk % 4
                        q_ = blk % 2
                        bnd = 4 + (blk // 2) % 2
                        for a in range(2):
                            for isden in range(2):
                                for c in range(2 if hasprev else 1):
                                    kblk = blk - c * d
                                    rhs_ = PT[sb_][:, (a * 2 + c) * 128:(a * 2 + c + 1) * 128]
                                    if not isden:
                                        op("pe", lambda e, a=a, c=c, kblk=kblk, rhs_=rhs_: e.matmul(bank(bnd)[a * 64:(a + 1) * 64, q_ * 128:(q_ + 1) * 128], lhsT=Vb[:, kblk, a * 64:(a + 1) * 64], rhs=rhs_, start=(c == 0), stop=(c == 1 or not hasprev)),
                                           reads=[R_Vb, R_PT[sb_]], writes=[RB[bnd]])
                                    else:
                                        op("pe", lambda e, a=a, c=c, rhs_=rhs_: e.matmul(bank(bnd)[a * 64:(a + 1) * 64, 256 + q_ * 128:256 + (q_ + 1) * 128], lhsT=ones_bf[:, :], rhs=rhs_, start=(c == 0), stop=(c == 1 or not hasprev)),
                                           reads=[R_ca, R_PT[sb_]], writes=[RB[bnd]])
                        if q_ == 1:
                            blk0 = blk - 1
                            if d == 1:
                                def view(acc):
                                    return acc[:, blk0 * 128:(blk0 + 2) * 128].rearrange("p (a q) -> p a q", a=2)
                            else:
                                n, r0 = blk0 // d, blk0 % d

                                def view(acc, n=n, r0=r0):
                                    return acc[:, n * 128 * d:(n + 1) * 128 * d].rearrange("p (q r) -> p r q", r=d)[:, r0:r0 + 2, :]
                            for acc, c0 in ((accN, 0), (accD, 256)):
                                pv = bank(bnd)[:, c0:c0 + 256].rearrange("p (a q) -> p a q", a=2)
                                if g == 0:
                                    op("dve", lambda e, acc=acc, pv=pv: e.tensor_copy(out=view(acc), in_=pv), reads=[RB[bnd]], writes=[R_acc])
                                else:
                                    op("dve", lambda e, acc=acc, pv=pv: e.tensor_tensor(out=view(acc), in0=view(acc), in1=pv, op=ALU.add), reads=[RB[bnd], R_acc], writes=[R_acc])

                    LA = 3
                    for blk in range(32 + LA):
                        if blk >= LA:
                            stage2(blk - LA)
                        if blk < 32:
                            stage1(blk)
                        issue_casts(1)
                    if g == 2:
                        op("dve", lambda e: e.reciprocal(out=accD[:], in_=accD[:]), reads=[R_acc], writes=[R_acc])
                        op("dve", lambda e: e.tensor_tensor(out=ybuf[:], in0=accN[:], in1=accD[:], op=ALU.mult), reads=[R_acc], writes=[R_yb])
                        op("sp", lambda e, hp=hp: e.dma_start(out=yattT_d[hp], in_=ybuf[:]), reads=[R_yb], writes=[R_yattd], dma="yatt")
                S.barrier()
            if stop_after == "A":
                return nc, "yattT_d", S

            with ExitStack() as esb:
                def sbb(name, shape, dt):
                    return esb.enter_context(nc.sbuf_tensor(name, list(shape), dt)).ap()

                tri_incl = sbb("tri_incl", [128, 128], F32)
                tri_after = sbb("tri_after", [128, 128], F32)
                sel2 = sbb("sel2", [128, 2], F32)
                cmk = sbb("cmk", [128, 2], F32)
                lbt = sbb("lbt", [128, D], F32)
                omlt = sbb("omlt", [128, D], F32)
                gaint = sbb("gaint", [128, D], F32)
                R_cb = Res()
                op("sp", lambda e: e.dma_start(out=tri_incl[:], in_=c_tri_incl[:, :]), writes=[R_cb], dma="c0")
                op("sp", lambda e: e.dma_start(out=tri_after[:], in_=c_tri_after[:, :]), writes=[R_cb], dma="c1")
                op("sp", lambda e: e.dma_start(out=sel2[:], in_=c_sel2[:, :]), writes=[R_cb], dma="c2")
                op("sp", lambda e: e.dma_start(out=cmk[:], in_=c_cm[:, :]), writes=[R_cb], dma="c6")
                op("sp", lambda e: e.dma_start(out=lbt[:], in_=v_lb0[:, :]), writes=[R_cb], dma="c3")
                op("sp", lambda e: e.dma_start(out=omlt[:], in_=v_lb1[:, :]), writes=[R_cb], dma="c4")
                op("sp", lambda e: e.dma_start(out=gaint[:], in_=v_gain[:, :]), writes=[R_cb], dma="c5")
                S.barrier()
                op("dve", lambda e: e.tensor_tensor(out=omlt[:], in0=omlt[:], in1=lbt[:], op=ALU.subtract), reads=[R_cb], writes=[R_cb])
                op("act", lambda e: e.activation(out=omlt[:], in_=omlt[:], func=AF.Exp), reads=[R_cb], writes=[R_cb])
                op("dve", lambda e: e.tensor_scalar_add(out=omlt[:], in0=omlt[:], scalar1=1.0), reads=[R_cb], writes=[R_cb])
                op("dve", lambda e: e.reciprocal(out=lbt[:], in_=omlt[:]), reads=[R_cb], writes=[R_cb])
                op("dve", lambda e: e.tensor_scalar(out=omlt[:], in0=lbt[:], scalar1=-1.0, scalar2=1.0, op0=ALU.mult, op1=ALU.add), reads=[R_cb], writes=[R_cb])
                S.barrier()

                whp = [sbb(f"whp{i}", [128, 8, 4, 256], BF16) for i in range(2)]
                R_whp = [Res(), Res()]
                YT = [sbb("YT0", [128, 2, T], BF16)] * 2
                R_YT = [Res()] * 2

                def mk(name, shape, dt, n):
                    return [sbb(f"{name}{i}", shape, dt) for i in range(n)], [Res() for _ in range(n)]

                Eb, R_Eb = mk("Eb", [128, 768], F32, 2)
                FG, R_FG = mk("FG", [128, 256], F32, 2)
                LF, R_LF = mk("LF", [128, 256], F32, 2)
                KEY, R_KEY = mk("KEY", [128, 256], F32, 2)
                Vv, R_V = mk("Vv", [128, 256], BF16, 5)
                SG, R_SG = mk("SG", [128, 256], F32, 5)
                EB, R_EB = mk("EB", [128, 512], F32, 2)
                ENB, R_ENB = mk("ENB", [128, 256], F32, 2)
                QD, R_QD = mk("QD", [128, 256], BF16, 2)
                KI, R_KI = mk("KI", [128, 256], BF16, 2)
                KE, R_KE = mk("KE", [128, 2, 256], BF16, 3)
                KEF, R_KEF = mk("KEF", [128, 256], F32, 2)
                DEC, R_DEC = mk("DEC", [128, 4], F32, 3)
                QKT, R_QKT = mk("QKT", [128, 4, 128], BF16, 3)
                AT, R_AT = mk("AT", [128, 2, 128], BF16, 3)
                YH, R_YH = mk("YH", [128, 256], BF16, 2)
                Y1, R_Y1 = mk("Y1", [128, 256], F32, 2)
                SS, R_SS = mk("SS", [128, 4], F32, 2)
                junk = sbb("junk", [128, 128], F32)
                R_junk = Res()
                ST = [sbb(f"ST{a}", [128, 128], F32) for a in range(2)]
                R_ST = [Res(), Res()]
                SBF = [[[sbb(f"SBF{a}{c}{u}", [128, 128], BF16) for u in range(3)] for c in range(2)] for a in range(2)]
                R_SBF = [[[Res() for u in range(3)] for c in range(2)] for a in range(2)]
                RB = PBanks()
                R_PB, R_U, R_TRqk, R_TRy, R_DECp, R_aT, R_o = RB[4], RB[5], RB[6], RB[6], RB[6], RB[7], RB[7]

                def PJ(i):
                    return ps[:, i * 1024:(i + 1) * 1024]

                def load_whp(hp):
                    i = hp % 2
                    for ty, base in enumerate((QH0, FH0, IH0, GH0)):
                        c0 = base + hp * 256
                        op("pool", lambda e, i=i, ty=ty, c0=c0: e.dma_start(out=whp[i][:, :, ty, :], in_=w_in[:, c0:c0 + 256].rearrange("(k p) j -> p k j", p=128)),
                           writes=[R_whp[i]], dma=f"whp{i}")

                if "B" not in skip:
                    load_whp(0)
                NTB = small["NTB"] if small and "NTB" in small else NT
                for hp in range(0 if "B" in skip else (small["HPS"] if small and "HPS" in small else 4)):
                    wi = hp % 2
                    if hp + 1 < 4:
                        load_whp(hp + 1)
                    hs = slice(hp * 256, (hp + 1) * 256)
                    for a in range(2):
                        op("dve", lambda e, a=a: e.memset(ST[a][:], 0.0), writes=[R_ST[a]])
                        op("dve", lambda e, a=a: e.memset(SBF[a][0][0][:], 0.0), writes=[R_SBF[a][0][0]])

                    def s0(t):
                        i = t % 2
                        for half in range(2):
                            for kc in range(8):
                                op("pe", lambda e, kc=kc, half=half, i=i, t=t: e.matmul(PJ(i)[:, half * 512:(half + 1) * 512], lhsT=xT[:, kc, t * 128:(t + 1) * 128],
                                                                                     rhs=whp[wi][:, kc, 2 * half:2 * half + 2, :].rearrange("p a j -> p (a j)"), start=(kc == 0), stop=(kc == 7)),
                                   reads=[R_xT, R_whp[wi]], writes=[RB[2 * i + half]])

                    def s1(t):
                        i = t % 2
                        op("act", lambda e, i=i: e.activation(out=Eb[i][:], in_=PJ(i)[:, 256:1024], func=AF.Exp, scale=-1.0), reads=[RB[2 * i], RB[2 * i + 1]], writes=[R_Eb[i]])
                        op("act", lambda e, i=i: e.activation(out=Eb[i][:], in_=Eb[i][:], func=AF.Ln, bias=1.0, scale=1.0), reads=[R_Eb[i]], writes=[R_Eb[i]])
                        op("act", lambda e, i=i: e.activation(out=Eb[i][:], in_=Eb[i][:], func=AF.Exp, scale=-1.0), reads=[R_Eb[i]], writes=[R_Eb[i]])
                        op("dve", lambda e, i=i: e.tensor_tensor(out=FG[i][:], in0=Eb[i][:, 0:256], in1=omlt[:, hs], op=ALU.mult), reads=[R_Eb[i], R_cb], writes=[R_FG[i]])
                        op("dve", lambda e, i=i: e.tensor_tensor(out=FG[i][:], in0=FG[i][:], in1=lbt[:, hs], op=ALU.add), reads=[R_FG[i], R_cb], writes=[R_FG[i]])
                        op("act", lambda e, i=i: e.activation(out=LF[i][:], in_=FG[i][:], func=AF.Ln), reads=[R_FG[i]], writes=[R_LF[i]])
                        op("dve", lambda e, i=i: e.tensor_scalar(out=KEY[i][:], in0=FG[i][:], scalar1=-1.0, scalar2=1.0, op0=ALU.mult, op1=ALU.add), reads=[R_FG[i]], writes=[R_KEY[i]])
                        op("dve", lambda e, i=i, t=t: e.tensor_tensor(out=Vv[t % 5][:], in0=PJ(i)[:, 512:768], in1=Eb[i][:, 256:512], op=ALU.mult), reads=[RB[2 * i + 1], R_Eb[i]], writes=[R_V[t % 5]])
                        op("dve", lambda e, i=i, t=t: e.tensor_tensor(out=SG[t % 5][:], in0=PJ(i)[:, 768:1024], in1=Eb[i][:, 512:768], op=ALU.mult), reads=[RB[2 * i + 1], R_Eb[i]], writes=[R_SG[t % 5]])

                    def s2(t):
                        i = t % 2
                        op("pe", lambda e, i=i: e.matmul(bank(4)[:, 0:256], lhsT=tri_incl[:], rhs=LF[i][:], start=True, stop=True), reads=[R_cb, R_LF[i]], writes=[R_PB])
                        op("pe", lambda e, i=i: e.matmul(bank(4)[:, 256:512], lhsT=tri_after[:], rhs=LF[i][:], start=True, stop=True), reads=[R_cb, R_LF[i]], writes=[R_PB])
                        op("act", lambda e, i=i: e.activation(out=EB[i][:], in_=bank(4), func=AF.Exp), reads=[R_PB], writes=[R_EB[i]])
                        op("act", lambda e, i=i: e.activation(out=ENB[i][:], in_=bank(4)[:, 0:256], func=AF.Exp, scale=-1.0), reads=[R_PB], writes=[R_ENB[i]])
                        op("dve", lambda e, i=i: e.tensor_tensor(out=QD[i][:], in0=PJ(i)[:, 0:256], in1=EB[i][:, 0:256], op=ALU.mult), reads=[RB[2 * i], R_EB[i]], writes=[R_QD[i]])
                        op("pool", lambda e, i=i: e.tensor_tensor(out=KI[i][:], in0=KEY[i][:], in1=ENB[i][:], op=ALU.mult), reads=[R_KEY[i], R_ENB[i]], writes=[R_KI[i]])
                        op("pool", lambda e, i=i: e.tensor_tensor(out=KEF[i][:], in0=KEY[i][:], in1=EB[i][:, 256:512], op=ALU.mult), reads=[R_KEY[i], R_EB[i]], writes=[R_KEF[i]])
                        for c in range(2):
                            op("act", lambda e, i=i, t=t, c=c: e.activation(out=KE[t % 3][:, c, :], in_=KEF[i][:], func=AF.Copy, scale=cmk[:, c:c + 1]), reads=[R_KEF[i], R_cb], writes=[R_KE[t % 3]])
                        for a in range(2):
                            op("pe", lambda e, i=i, a=a: e.matmul(bank(6)[:, 448 + 2 * a:450 + 2 * a], lhsT=EB[i][:, a * 128:(a + 1) * 128], rhs=sel2[:], start=True, stop=True), reads=[R_EB[i], R_cb], writes=[R_DECp])
                        op("dve", lambda e, t=t: e.tensor_copy(out=DEC[t % 3][:], in_=bank(6)[:, 448:452]), reads=[R_DECp], writes=[R_DEC[t % 3]])

                    def s3(t):
                        i = t % 2
                        j = t % 3
                        tb = bankbf(6)
                        for a in range(2):
                            op("pe", lambda e, i=i, a=a: e.transpose(out=tb[:, a * 128:(a + 1) * 128], in_=QD[i][:, a * 128:(a + 1) * 128], identity=ident[:]), reads=[R_QD[i], R_c], writes=[R_TRqk])
                            op("pe", lambda e, i=i, a=a: e.transpose(out=tb[:, (2 + a) * 128:(3 + a) * 128], in_=KI[i][:, a * 128:(a + 1) * 128], identity=ident[:]), reads=[R_KI[i], R_c], writes=[R_TRqk])
                        op("act", lambda e, j=j: e.copy(out=QKT[j][:], in_=tb[:, 0:512].rearrange("p (a q) -> p a q", a=4)), reads=[R_TRqk], writes=[R_QKT[j]])
                        for a in range(2):
                            op("pe", lambda e, j=j, a=a: e.matmul(bank(7)[:, a * 128:(a + 1) * 128], lhsT=QKT[j][:, 2 + a, :], rhs=QKT[j][:, a, :], start=True, stop=True), reads=[R_QKT[j]], writes=[R_aT])
                        for a in range(2):
                            op("dve", lambda e, j=j, a=a: e.tensor_tensor(out=AT[j][:, a, :], in0=bank(7)[:, a * 128:(a + 1) * 128], in1=tri_incl[:], op=ALU.mult), reads=[R_aT, R_cb], writes=[R_AT[j]])

                    def s4(t):
                        for a in range(2):
                            for c in range(2):
                                op("pe", lambda e, a=a, c=c, t=t: e.matmul(bank(5)[:, (a * 2 + c) * 128:(a * 2 + c + 1) * 128], lhsT=KE[t % 3][:, c, a * 128:(a + 1) * 128],
                                                                         rhs=Vv[t % 5][:, a * 128:(a + 1) * 128], start=True, stop=True),
                                   reads=[R_KE[t % 3], R_V[t % 5]], writes=[R_U])
                        lvl = small.get("S4", 3) if small else 3
                        for c in range(2 if lvl >= 2 else 0):
                            for a in range(2):
                                op("dve", lambda e, a=a, c=c, t=t: e.scalar_tensor_tensor(out=ST[a][:], in0=ST[a][:], scalar=DEC[t % 3][:, 2 * a + c:2 * a + c + 1], in1=bank(5)[:, (a * 2 + c) * 128:(a * 2 + c + 1) * 128], op0=ALU.mult, op1=ALU.add),
                                   reads=[R_ST[a], R_DEC[t % 3], R_U], writes=[R_ST[a]])
                                u_ = (t % 3) if c == 0 else ((t + 1) % 3)
                                if lvl < 3:
                                    continue
                                op("dve", lambda e, a=a, c=c, u_=u_: e.tensor_copy(out=SBF[a][1 - c][u_][:], in_=ST[a][:]), reads=[R_ST[a]], writes=[R_SBF[a][1 - c][u_]])

                    def s5(t):
                        j = t % 3
                        i = t % 2
                        for a in range(2):
                            o_ = bank(7)[:, 256 + a * 128:256 + (a + 1) * 128]
                            op("pe", lambda e, a=a, j=j, t=t, o_=o_: e.matmul(o_, lhsT=AT[j][:, a, :], rhs=Vv[t % 5][:, a * 128:(a + 1) * 128], start=True, stop=False), reads=[R_AT[j], R_V[t % 5]], writes=[R_o])
                            for c in range(2):
                                op("pe", lambda e, a=a, c=c, j=j, o_=o_, t=t: e.matmul(o_[c * 64:(c + 1) * 64, :], lhsT=QKT[j][:, a, c * 64:(c + 1) * 64], rhs=SBF[a][c][t % 3][:], start=False, stop=True),
                                   reads=[R_QKT[j], R_SBF[a][c][t % 3]], writes=[R_o])
                        op("pool", lambda e, i=i: e.memset(SS[i][:, 0:2], 0.0), writes=[R_SS[i]])
                        for a in range(2):
                            op("act", lambda e, a=a, i=i: e.activation(out=junk[:], in_=bank(7)[:, 256 + a * 128:256 + (a + 1) * 128], func=AF.Square, accum_out=SS[i][:, a:a + 1]), reads=[R_o], writes=[R_SS[i], R_junk])
                        op("act", lambda e, i=i: e.activation(out=SS[i][:, 2:4], in_=SS[i][:, 0:2], func=AF.Ln, scale=1.0 / 128.0, bias=RMS_EPS), reads=[R_SS[i]], writes=[R_SS[i]])
                        op("act", lambda e, i=i: e.activation(out=SS[i][:, 2:4], in_=SS[i][:, 2:4], func=AF.Exp, scale=-0.5), reads=[R_SS[i]], writes=[R_SS[i]])
                        for a in range(2):
                            op("dve", lambda e, a=a, i=i: e.scalar_tensor_tensor(out=Y1[i][:, a * 128:(a + 1) * 128], in0=bank(7)[:, 256 + a * 128:256 + (a + 1) * 128], scalar=SS[i][:, 2 + a:3 + a],
                                                                              in1=gaint[:, hp * 256 + a * 128:hp * 256 + (a + 1) * 128], op0=ALU.mult, op1=ALU.mult),
                               reads=[R_o, R_SS[i], R_cb], writes=[R_Y1[i]])
                        op("pool", lambda e, i=i, t=t: e.tensor_tensor(out=YH[i][:], in0=Y1[i][:], in1=SG[t % 5][:], op=ALU.mult), reads=[R_Y1[i], R_SG[t % 5]], writes=[R_YH[i]])

                    def s6(t):
                        i = t % 2
                        tb = bankbf(6)
                        for a in range(2):
                            op("pe", lambda e, i=i, a=a: e.transpose(out=tb[:, 512 + a * 128:512 + (a + 1) * 128], in_=YH[i][:, a * 128:(a + 1) * 128], identity=ident[:]), reads=[R_YH[i], R_c], writes=[R_TRy])
                        op("act", lambda e, t=t: e.copy(out=YT[wi][:, :, t * 128:(t + 1) * 128], in_=tb[:, 512:768].rearrange("p (a q) -> p a q", a=2)), reads=[R_TRy], writes=[R_YT[wi]])

                    stages = [s0, s1, s2, s3, s4, s5, s6]
                    if small and "NST" in small:
                        stages = stages[:small["NST"]]
                    for jj in range(NTB + len(stages) - 1):
                        lists = []
                        for k in reversed(range(1, len(stages))):
                            t = jj - k
                            if 0 <= t < NTB:
                                S.defer = []
                                stages[k](t)
                                lists.append(S.defer)
                                S.defer = None
                        while any(lists):
                            for L in lists:
                                if L:
                                    op(*L.pop(0))
                        if 0 <= jj < NTB:
                            stages[0](jj)
                        issue_casts(1)
                    for a in range(2):
                        op("sp", lambda e, a=a, hp=hp: e.dma_start(out=yhgT_d[2 * hp + a][:, 0:NTB * 128], in_=YT[wi][:, a, 0:NTB * 128]), reads=[R_YT[wi]], writes=[R_yhgd], dma=f"yhg{wi}")
                S.barrier()
        if stop_after == "B":
            return nc, "yhgT_d", S
        if "W" not in skip:
            issue_casts(len(castq))

        with ExitStack() as esc:
            def sbc(name, shape, dt):
                return esc.enter_context(nc.sbuf_tensor(name, list(shape), dt)).ap()

            wga = sbc("wga", [128, 8, D], BF16)
            wgh = sbc("wgh", [128, 8, D], BF16)
            wa_ = sbc("wa_", [128, 4, D], BF16)
            wh_ = sbc("wh_", [128, 8, D], BF16)
            wo_ = sbc("wo_", [128, 8, D], BF16)
            wr32 = sbc("wr32", [128, 8, NE], F32)
            ln1w = sbc("ln1w", [128, D], F32)
            ln1b = sbc("ln1b", [128, D], F32)
            rbt = sbc("rbt", [128, NE], F32)
            R_cc = Res()
            op("pool", lambda e: e.dma_start(out=wga[:], in_=w_in[:, GA0:GA0 + D].rearrange("(k p) j -> p k j", p=128)), writes=[R_cc], dma="pc0")
            op("pool", lambda e: e.dma_start(out=wgh[:], in_=w_in[:, GHG0:GHG0 + D].rearrange("(k p) j -> p k j", p=128)), writes=[R_cc], dma="pc1")
            op("pool", lambda e: e.dma_start(out=wa_[:], in_=w_a.rearrange("(k p) j -> p k j", p=128)), writes=[R_cc], dma="pc2")
            op("pool", lambda e: e.dma_start(out=wh_[:], in_=w_h.rearrange("(k p) j -> p k j", p=128)), writes=[R_cc], dma="pc3")
            op("pool", lambda e: e.dma_start(out=wo_[:], in_=w_o.rearrange("(k p) j -> p k j", p=128)), writes=[R_cc], dma="pc4")
            op("sp", lambda e: e.dma_start(out=wr32[:], in_=w_r.rearrange("(k p) j -> p k j", p=128)), writes=[R_cc], dma="c5")
            op("sp", lambda e: e.dma_start(out=ln1w[:], in_=v_ln1w[:, :]), writes=[R_cc], dma="c6")
            op("sp", lambda e: e.dma_start(out=ln1b[:], in_=v_ln1b[:, :]), writes=[R_cc], dma="c7")
            op("sp", lambda e: e.dma_start(out=rbt[:], in_=v_rb[:, :]), writes=[R_cc], dma="c8")
            S.barrier()

            def mk(name, shape, dt, n):
                return [sbc(f"{name}{i}", shape, dt) for i in range(n)], [Res() for _ in range(n)]

            xTs, R_xTs = mk("xTs", [128, 8, 512], BF16, 1)
            yas, R_yas = mk("yas", [128, 4, 512], BF16, 1)
            yhs, R_yhs = mk("yhs", [128, 8, 512], BF16, 1)
            xs, R_xs = mk("xs", [128, D], F32, 2)
            xTs, R_xTs, yas, R_yas, yhs, R_yhs = xTs * 2, R_xTs * 2, yas * 2, R_yas * 2, yhs * 2, R_yhs * 2
            mT, R_mT = mk("mT", [128, 8, 512], BF16, 1)
            EA, R_EA = mk("EA", [128, 1024], F32, 2)
            TM, R_TM = mk("TM", [128, 1024], F32, 2)
            Z, R_Z = mk("Z", [128, D], F32, 2)
            X1, R_X1 = mk("X1", [128, D], F32, 2)
            X1T32, R_X1T32 = mk("X1T32", [128, 8, 128], F32, 2)
            X1Tb, R_X1Tb = mk("X1Tb", [128, 8, 512], BF16, 1)
            X1Tb, R_X1Tb = X1Tb * 2, R_X1Tb * 2
            st6, R_st6 = mk("st6", [128, 12], F32, 2)
            mv, R_mv = mk("mv", [128, 4], F32, 2)
            RT, R_RT = mk("RT", [128, 6, NE], F32, 2)
            r8, R_r8 = mk("r8", [128, 10, 8], F32, 2)
            Gs, R_Gs = mk("Gs", [128, 4, NE], F32, 2)
            RB = PBanks()

            def load_slab(s):
                i = s % 2
                sl = slice(s * 512, (s + 1) * 512)
                op("sp", lambda e, i=i: e.dma_start(out=xTs[i][:], in_=xT_d[:, :, sl].rearrange("k p t -> p k t")), reads=[R_xTd], writes=[R_xTs[i]], dma=f"l0{i}")
                op("sp", lambda e, i=i: e.dma_start(out=yas[i][:], in_=yattT_d[:, :, sl].rearrange("k p t -> p k t")), reads=[R_yattd], writes=[R_yas[i]], dma=f"l1{i}")
                op("sp", lambda e, i=i: e.dma_start(out=yhs[i][:], in_=yhgT_d[:, :, sl].rearrange("k p t -> p k t")), reads=[R_yhgd], writes=[R_yhs[i]], dma=f"l2{i}")

            NSC = small["NSC"] if small and "NSC" in small else 8
            if "C" in skip:
                NSC = 0
            for s in range(NSC):
                i = s % 2
                load_slab(s)
                for dc in range(8):
                    b0 = 4 * (dc % 2)
                    cs = slice(dc * 128, (dc + 1) * 128)
                    for kc in range(8):
                        op("pe", lambda e, kc=kc, b0=b0, cs=cs: e.matmul(bank(b0), lhsT=wga[:, kc, cs], rhs=xTs[i][:, kc, :], start=(kc == 0), stop=(kc == 7)), reads=[R_cc, R_xTs[i]], writes=[RB[b0]])
                    for kc in range(8):
                        op("pe", lambda e, kc=kc, b0=b0, cs=cs: e.matmul(bank(b0 + 1), lhsT=wgh[:, kc, cs], rhs=xTs[i][:, kc, :], start=(kc == 0), stop=(kc == 7)), reads=[R_cc, R_xTs[i]], writes=[RB[b0 + 1]])
                    for kc in range(4):
                        op("pe", lambda e, kc=kc, b0=b0, cs=cs: e.matmul(bank(b0 + 2), lhsT=wa_[:, kc, cs], rhs=yas[i][:, kc, :], start=(kc == 0), stop=(kc == 3)), reads=[R_cc, R_yas[i]], writes=[RB[b0 + 2]])
                    for kc in range(8):
                        op("pe", lambda e, kc=kc, b0=b0, cs=cs: e.matmul(bank(b0 + 3), lhsT=wh_[:, kc, cs], rhs=yhs[i][:, kc, :], start=(kc == 0), stop=(kc == 7)), reads=[R_cc, R_yhs[i]], writes=[RB[b0 + 3]])
                    j = dc % 2
                    op("act", lambda e, b0=b0, j=j: e.activation(out=EA[j][:], in_=ps[:, b0 * 512:(b0 + 2) * 512], func=AF.Exp, scale=-1.0), reads=[RB[b0], RB[b0 + 1]], writes=[R_EA[j]])
                    op("act", lambda e, j=j: e.activation(out=EA[j][:], in_=EA[j][:], func=AF.Ln, bias=1.0, scale=1.0), reads=[R_EA[j]], writes=[R_EA[j]])
                    op("act", lambda e, j=j: e.activation(out=EA[j][:], in_=EA[j][:], func=AF.Exp, scale=-1.0), reads=[R_EA[j]], writes=[R_EA[j]])
                    op("dve", lambda e, b0=b0, j=j: e.tensor_tensor(out=TM[j][:], in0=EA[j][:], in1=ps[:, (b0 + 2) * 512:(b0 + 4) * 512], op=ALU.mult), reads=[R_EA[j], RB[b0 + 2], RB[b0 + 3]], writes=[R_TM[j]])
                    op("dve", lambda e, j=j, dc=dc: e.tensor_tensor(out=mT[0][:, dc, :], in0=TM[j][:, 0:512], in1=TM[j][:, 512:1024], op=ALU.add), reads=[R_TM[j]], writes=[R_mT[0]])
                def sub_chain(sub):
                        t = s * 4 + sub
                        j = t % 2
                        bo = 2 * (sub % 2)
                        op("sp", lambda e, j=j, t=t: e.dma_start(out=xs[j][:], in_=x_d[t * 128:(t + 1) * 128, :]), writes=[R_xs[j]], dma=f"l3{j}")
                        for half in range(2):
                            for kc in range(8):
                                op("pe", lambda e, kc=kc, half=half, bo=bo, sub=sub: e.matmul(bank(bo + half), lhsT=mT[0][:, kc, sub * 128:(sub + 1) * 128], rhs=wo_[:, kc, half * 512:(half + 1) * 512], start=(kc == 0), stop=(kc == 7)),
                                   reads=[R_mT[0], R_cc], writes=[RB[bo + half]])
                        op("dve", lambda e, j=j, bo=bo, sub=sub: e.scalar_tensor_tensor(out=Z[j][:], in0=xs[j][:], scalar=ALPHA, in1=ps[:, bo * 512:(bo + 2) * 512], op0=ALU.mult, op1=ALU.add),
                           reads=[R_xs[j], RB[bo], RB[bo + 1]], writes=[R_Z[j]])
                        emit_ln(op, Z[j], R_Z[j], X1[j], R_X1[j], st6[j], R_st6[j], mv[j], R_mv[j], ln1w, ln1b, R_cc, "dve")
                        op("sp", lambda e, j=j, t=t: e.dma_start(out=x1_d[t * 128:(t + 1) * 128, :], in_=X1[j][:]), reads=[R_X1[j]], writes=[R_x1d], dma=f"x1o{j}")
                        bt = 4 + 2 * (sub % 2)
                        for kc in range(8):
                            op("pe", lambda e, kc=kc, bt=bt, j=j: e.transpose(out=ps[:, bt * 512 + kc * 128:bt * 512 + (kc + 1) * 128], in_=X1[j][:, kc * 128:(kc + 1) * 128], identity=identf[:]),
                               reads=[R_X1[j], R_c], writes=[RB[bt], RB[bt + 1]])
                        op("act", lambda e, bt=bt, j=j: e.copy(out=X1T32[j][:], in_=ps[:, bt * 512:(bt + 2) * 512].rearrange("p (k q) -> p k q", k=8)), reads=[RB[bt], RB[bt + 1]], writes=[R_X1T32[j]])
                        op("act", lambda e, j=j, sub=sub: e.copy(out=X1Tb[i][:, :, sub * 128:(sub + 1) * 128], in_=X1T32[j][:]), reads=[R_X1T32[j]], writes=[R_X1Tb[i]])
                        for kc in range(8):
                            op("pe", lambda e, kc=kc, j=j, bt=bt: e.matmul(bank(bt)[:, 0:NE], lhsT=X1T32[j][:, kc, :], rhs=wr32[:, kc, :], start=(kc == 0), stop=(kc == 7)), reads=[R_X1T32[j], R_cc], writes=[RB[bt], RB[bt + 1]])
                        emit_route(op, bank(bt)[:, 0:NE], [RB[bt], RB[bt + 1]], RT[j], R_RT[j], r8[j], R_r8[j], rbt, R_cc, Gs[i][:, sub, :], R_Gs[i])

                for pair in ((0, 1), (2, 3)):
                    lists = []
                    for sub in pair:
                        S.defer = []
                        sub_chain(sub)
                        lists.append(S.defer)
                        S.defer = None
                    while any(lists):
                        for L in lists:
                            if L:
                                op(*L.pop(0))
                op("sp", lambda e, i=i, s=s: e.dma_start(out=x1T_d[:, :, s * 512:(s + 1) * 512].rearrange("k p t -> p k t"), in_=X1Tb[i][:]), reads=[R_X1Tb[i]], writes=[R_x1Td], dma=f"x1T{i}")
                op("sp", lambda e, i=i, s=s: e.dma_start(out=G_d[s * 512:(s + 1) * 512, :].rearrange("(a p) j -> p a j", p=128), in_=Gs[i][:]), reads=[R_Gs[i]], writes=[R_Gd], dma=f"G{i}")
            S.barrier()
        if stop_after == "C":
            return nc, "x1_d", S

        with ExitStack() as esd:
            def sbd(name, shape, dt):
                return esd.enter_context(nc.sbuf_tensor(name, list(shape), dt)).ap()

            wpg = sbd("wpg", [128, 8, D], BF16)
            wpp = sbd("wpp", [128, 2, D], BF16)
            ln2w = sbd("ln2w", [128, D], F32)
            ln2b = sbd("ln2b", [128, D], F32)
            R_cd = Res()
            op("pool", lambda e: e.dma_start(out=wpg[:], in_=w_pg.rearrange("(k p) j -> p k j", p=128)), writes=[R_cd], dma="pc0")
            op("pool", lambda e: e.dma_start(out=wpp[:], in_=w_pp.rearrange("(k p) j -> p k j", p=128)), writes=[R_cd], dma="pc1")
            op("sp", lambda e: e.dma_start(out=ln2w[:], in_=v_ln2w[:, :]), writes=[R_cd], dma="c2")
            op("sp", lambda e: e.dma_start(out=ln2b[:], in_=v_ln2b[:, :]), writes=[R_cd], dma="c3")
            S.barrier()

            def mk(name, shape, dt, n):
                return [sbd(f"{name}{i}", shape, dt) for i in range(n)], [Res() for _ in range(n)]

            NWB = 3
            WG, R_WG = mk("WG", [128, 8, 256], BF16, NWB)
            WU, R_WU = mk("WU", [128, 8, 256], BF16, NWB)
            WD, R_WD = mk("WD", [128, 2, D], BF16, NWB)
            hT, R_hT = mk("hT", [128, 8, 512], BF16, 2)
            x1s, R_x1s = mk("x1s", [128, 4, D], F32, 2)
            Gl, R_Gl = mk("Gl", [128, 4, NE], F32, 2)
            pbf, R_pbf = mk("pbf", [128, 4, 256], BF16, 2)
            pT, R_pT = mk("pT", [128, 2, 512], BF16, 1)
            yacc, R_yacc = mk("yacc", [128, 4, D], F32, 2)
            SGt, R_SGt = mk("SGt", [128, 512], F32, 2)
            ACTt, R_ACTt = mk("ACTt", [128, 512], BF16, 4)
            EP, R_EP = mk("EP", [128, D], F32, 2)
            OUT, R_OUT = mk("OUT", [128, D], F32, 2)
            st6, R_st6 = mk("st6d", [128, 12], F32, 2)
            mv, R_mv = mk("mvd", [128, 4], F32, 2)
            RB = PBanks()
            NEX = NE + 1
            NSD = small["NSD"] if small and "NSD" in small else 8
            seq = [(s, e_) for s in range(NSD) for e_ in range(NEX)]

            def load_w(n):
                s, e_ = seq[n]
                b = n % NWB
                op("sp", lambda e, b=b, e_=e_: e.dma_start(out=WG[b][:], in_=wg_d[e_].rearrange("p (k j) -> p k j", k=8)), reads=[R_wcast], writes=[R_WG[b]], dma=f"wg{b}")
                op("sp", lambda e, b=b, e_=e_: e.dma_start(out=WU[b][:], in_=wu_d[e_].rearrange("p (k j) -> p k j", k=8)), reads=[R_wcast], writes=[R_WU[b]], dma=f"wu{b}")
                op("sp", lambda e, b=b, e_=e_: e.dma_start(out=WD[b][:], in_=wd_d[e_].rearrange("p (k j) -> p k j", k=2)), reads=[R_wcast], writes=[R_WD[b]], dma=f"wd{b}")

            def load_slab(s):
                i = s % 2
                sl = slice(s * 512, (s + 1) * 512)
                op("sp", lambda e, i=i: e.dma_start(out=hT[i][:], in_=x1T_d[:, :, sl].rearrange("k p t -> p k t")), reads=[R_x1Td], writes=[R_hT[i]], dma=f"m0{i}")
                op("sp", lambda e, i=i: e.dma_start(out=x1s[i][:], in_=x1_d[sl, :].rearrange("(a p) j -> p a j", p=128)), reads=[R_x1d], writes=[R_x1s[i]], dma=f"m1{i}")
                op("sp", lambda e, i=i: e.dma_start(out=Gl[i][:], in_=G_d[sl, :].rearrange("(a p) j -> p a j", p=128)), reads=[R_Gd], writes=[R_Gl[i]], dma=f"m2{i}")
                op("pool", lambda e, i=i: e.dma_start(out=pbf[i][:], in_=p_d[sl, :].rearrange("(a p) j -> p a j", p=128)), writes=[R_pbf[i]], dma=f"m3{i}")

            load_slab(0)
            load_w(0)
            load_w(1)

            def gu_gate(n, c):
                s, e_ = seq[n]
                b = n % NWB
                i = s % 2
                for kc in range(8):
                    op("pe", lambda e, kc=kc, b=b, c=c, i=i: e.matmul(bank(2 * c), lhsT=WG[b][:, kc, c * 128:(c + 1) * 128], rhs=hT[i][:, kc, :], start=(kc == 0), stop=(kc == 7)), reads=[R_WG[b], R_hT[i]], writes=[RB[2 * c]])
                op("act", lambda e, c=c: e.activation(out=SGt[c][:], in_=bank(2 * c), func=AF.Silu), reads=[RB[2 * c]], writes=[R_SGt[c]])

            def gu_up(n, c):
                s, e_ = seq[n]
                b = n % NWB
                i = s % 2
                for kc in range(8):
                    op("pe", lambda e, kc=kc, b=b, c=c, i=i: e.matmul(bank(2 * c + 1), lhsT=WU[b][:, kc, c * 128:(c + 1) * 128], rhs=hT[i][:, kc, :], start=(kc == 0), stop=(kc == 7)), reads=[R_WU[b], R_hT[i]], writes=[RB[2 * c + 1]])
                a_ = (n % 2) * 2 + c
                op("dve", lambda e, c=c, a_=a_: e.tensor_tensor(out=ACTt[a_][:], in0=SGt[c][:], in1=bank(2 * c + 1), op=ALU.mult), reads=[R_SGt[c], RB[2 * c + 1]], writes=[R_ACTt[a_]])

            def gu(n, c):
                gu_gate(n, c)
                gu_up(n, c)

            def down_sub(n, sub):
                s, e_ = seq[n]
                b = n % NWB
                i = s % 2
                bo = 4 + 2 * (sub % 2)
                for half in range(2):
                    for c in range(2):
                        a_ = (n % 2) * 2 + c
                        op("pe", lambda e, c=c, a_=a_, half=half, bo=bo, sub=sub, b=b: e.matmul(bank(bo + half), lhsT=ACTt[a_][:, sub * 128:(sub + 1) * 128], rhs=WD[b][:, c, half * 512:(half + 1) * 512], start=(c == 0), stop=(c == 1)),
                           reads=[R_ACTt[a_], R_WD[b]], writes=[RB[bo + half]])
                if e_ < NE:
                    op("dve", lambda e, sub=sub, bo=bo, e_=e_, i=i: e.scalar_tensor_tensor(out=yacc[i][:, sub, :], in0=ps[:, bo * 512:(bo + 2) * 512], scalar=Gl[i][:, sub, e_:e_ + 1], in1=yacc[i][:, sub, :], op0=ALU.mult, op1=ALU.add),
                       reads=[RB[bo], RB[bo + 1], R_Gl[i], R_yacc[i]], writes=[R_yacc[i]])
                else:
                    op("dve", lambda e, sub=sub, bo=bo, i=i: e.tensor_tensor(out=yacc[i][:, sub, :], in0=ps[:, bo * 512:(bo + 2) * 512], in1=yacc[i][:, sub, :], op=ALU.add),
                       reads=[RB[bo], RB[bo + 1], R_yacc[i]], writes=[R_yacc[i]])

            def slab_prologue(s):
                i = s % 2
                tb = bankbf(6)
                for a in range(4):
                    for k in range(2):
                        op("pe", lambda e, a=a, k=k, i=i: e.transpose(out=tb[:, k * 512 + a * 128:k * 512 + (a + 1) * 128], in_=pbf[i][:, a, k * 128:(k + 1) * 128], identity=ident[:]), reads=[R_pbf[i], R_c], writes=[RB[6]])
                op("act", lambda e: e.copy(out=pT[0][:], in_=tb[:, 0:1024].rearrange("p (k t) -> p k t", k=2)), reads=[RB[6]], writes=[R_pT[0]])
                for sub in range(4):
                    op("act", lambda e, sub=sub, i=i: e.mul(out=yacc[i][:, sub, :], in_=x1s[i][:, sub, :], mul=ALPHA), reads=[R_x1s[i]], writes=[R_yacc[i]])
                for sub in range(4):
                    gb = 4 if sub % 2 == 0 else 0
                    ep = EP[sub % 2]
                    R_ep = R_EP[sub % 2]
                    for half in range(2):
                        for kc in range(8):
                            op("pe", lambda e, kc=kc, half=half, sub=sub, i=i, gb=gb: e.matmul(bank(gb + half), lhsT=hT[i][:, kc, sub * 128:(sub + 1) * 128], rhs=wpg[:, kc, half * 512:(half + 1) * 512], start=(kc == 0), stop=(kc == 7)), reads=[R_hT[i], R_cd], writes=[RB[gb + half]])
                        for kc in range(2):
                            op("pe", lambda e, kc=kc, half=half, sub=sub, gb=gb: e.matmul(bank(gb + 2 + half), lhsT=pT[0][:, kc, sub * 128:(sub + 1) * 128], rhs=wpp[:, kc, half * 512:(half + 1) * 512], start=(kc == 0), stop=(kc == 1)), reads=[R_pT[0], R_cd], writes=[RB[gb + 2 + half]])
                    op("act", lambda e, gb=gb, ep=ep: e.activation(out=ep[:], in_=ps[:, gb * 512:(gb + 2) * 512], func=AF.Exp, scale=-1.0), reads=[RB[gb], RB[gb + 1]], writes=[R_ep])
                    op("act", lambda e, ep=ep: e.activation(out=ep[:], in_=ep[:], func=AF.Ln, bias=1.0, scale=1.0), reads=[R_ep], writes=[R_ep])
                    op("act", lambda e, ep=ep: e.activation(out=ep[:], in_=ep[:], func=AF.Exp, scale=-1.0), reads=[R_ep], writes=[R_ep])
                    op("dve", lambda e, gb=gb, ep=ep: e.tensor_tensor(out=ep[:], in0=ep[:], in1=ps[:, (gb + 2) * 512:(gb + 4) * 512], op=ALU.mult), reads=[R_ep, RB[gb + 2], RB[gb + 3]], writes=[R_ep])
                    op("dve", lambda e, sub=sub, ep=ep, i=i: e.tensor_tensor(out=yacc[i][:, sub, :], in0=yacc[i][:, sub, :], in1=ep[:], op=ALU.add), reads=[R_ep, R_yacc[i]], writes=[R_yacc[i]])

            def slab_epilogue_sub(s, sub):
                t = s * 4 + sub
                j = t % 2
                i = s % 2
                emit_ln(op, yacc[i][:, sub, :], R_yacc[i], OUT[j], R_OUT[j], st6[j], R_st6[j], mv[j], R_mv[j], ln2w, ln2b, R_cd, "dve", in_readonly=True)
                op("sp", lambda e, j=j, t=t: e.dma_start(out=out_d[t * 128:(t + 1) * 128, :], in_=OUT[j][:]), reads=[R_OUT[j]], dma=f"out{j}")

            N = len(seq)
            for n in range(N):
                s, e_ = seq[n]
                if e_ == 0:
                    if s + 1 < NSD:
                        load_slab(s + 1)
                    slab_prologue(s)
                    gu(n, 0)
                    gu(n, 1)
                if n + 2 < N:
                    load_w(n + 2)
                nxt_same_slab = (n + 1 < N) and seq[n + 1][0] == s
                if nxt_same_slab:
                    gu_gate(n + 1, 0)
                    down_sub(n, 0)
                    gu_up(n + 1, 0)
                    down_sub(n, 1)
                    gu_gate(n + 1, 1)
                    down_sub(n, 2)
                    gu_up(n + 1, 1)
                    down_sub(n, 3)
                else:
                    for sub in range(4):
                        down_sub(n, sub)
                if s >= 1 and 2 <= e_ < 6:
                    slab_epilogue_sub(s - 1, e_ - 2)
                if n == N - 1:
                    for sub in range(4):
                        slab_epilogue_sub(s, sub)
            S.barrier()
    return nc, "out", S


def emit_ln(op, zin, R_zin, xout, R_xout, st6, R_st6, mv, R_mv, lw, lb, R_const, eng, in_readonly=False):
    for hf in range(2):
        op("dve", lambda e, hf=hf: e.bn_stats(out=st6[:, hf * 6:(hf + 1) * 6], in_=zin[:, hf * 512:(hf + 1) * 512]), reads=[R_zin], writes=[R_st6])
    op("dve", lambda e: e.bn_aggr(out=mv[:, 0:2], in_=st6[:]), reads=[R_st6], writes=[R_mv])
    op("act", lambda e: e.activation(out=mv[:, 2:3], in_=mv[:, 1:2], func=AF.Ln, bias=LN_EPS, scale=1.0), reads=[R_mv], writes=[R_mv])
    op("act", lambda e: e.activation(out=mv[:, 2:3], in_=mv[:, 2:3], func=AF.Exp, scale=-0.5), reads=[R_mv], writes=[R_mv])
    op("dve", lambda e: e.tensor_scalar(out=xout[:], in0=zin, scalar1=mv[:, 0:1], scalar2=mv[:, 2:3], op0=ALU.subtract, op1=ALU.mult), reads=[R_zin, R_mv], writes=[R_xout])
    op("dve", lambda e: e.tensor_tensor(out=xout[:], in0=xout[:], in1=lw[:], op=ALU.mult), reads=[R_xout, R_const], writes=[R_xout])
    op("dve", lambda e: e.tensor_tensor(out=xout[:], in0=xout[:], in1=lb[:], op=ALU.add), reads=[R_xout, R_const], writes=[R_xout])


def emit_route(op, logits, R_logits, RT, R_RT, r8, R_r8, rbt, R_const, Gout, R_Gout):
    BIG = 1.0e4
    s_ = RT[:, 0, :]
    sel = RT[:, 1, :]
    tmp = RT[:, 2, :]
    selm = RT[:, 3, :]
    em = RT[:, 4, :]
    g_ = RT[:, 5, :]
    op("act", lambda e: e.activation(out=s_, in_=logits, func=AF.Exp, scale=-1.0), reads=R_logits, writes=[R_RT])
    op("act", lambda e: e.activation(out=s_, in_=s_, func=AF.Ln, bias=1.0, scale=1.0), reads=[R_RT], writes=[R_RT])
    op("act", lambda e: e.activation(out=s_, in_=s_, func=AF.Exp, scale=-1.0), reads=[R_RT], writes=[R_RT])
    op("dve", lambda e: e.tensor_tensor(out=sel, in0=s_, in1=rbt[:], op=ALU.add), reads=[R_RT, R_const], writes=[R_RT])
    for gidx in range(8):
        op("dve", lambda e, gidx=gidx: e.max(out=r8[:, gidx, :], in_=sel[:, gidx * 8:(gidx + 1) * 8]), reads=[R_RT], writes=[R_r8])
    op("dve", lambda e: e.tensor_tensor(out=r8[:, 8, :], in0=r8[:, 0:8, 0], in1=r8[:, 0:8, 1], op=ALU.add), reads=[R_r8], writes=[R_r8])
    op("dve", lambda e: e.max(out=r8[:, 9, :], in_=r8[:, 8, :]), reads=[R_r8], writes=[R_r8])
    op("dve", lambda e: e.tensor_scalar(out=r8[:, 8, :], in0=r8[:, 8, :], scalar1=r8[:, 9, 3:4], scalar2=None, op0=ALU.is_ge), reads=[R_r8], writes=[R_r8])
    op("dve", lambda e: e.tensor_tensor(out=selm.rearrange("p (g j) -> p g j", g=8), in0=sel.rearrange("p (g j) -> p g j", g=8), in1=r8[:, 8, :].unsqueeze(2).to_broadcast([128, 8, 8]), op=ALU.mult), reads=[R_RT, R_r8], writes=[R_RT])
    op("dve", lambda e: e.tensor_scalar(out=r8[:, 8, :], in0=r8[:, 8, :], scalar1=-1.0, scalar2=BIG, op0=ALU.add, op1=ALU.mult), reads=[R_r8], writes=[R_r8])
    op("dve", lambda e: e.tensor_tensor(out=selm.rearrange("p (g j) -> p g j", g=8), in0=selm.rearrange("p (g j) -> p g j", g=8), in1=r8[:, 8, :].unsqueeze(2).to_broadcast([128, 8, 8]), op=ALU.add), reads=[R_RT, R_r8], writes=[R_RT])
    op("dve", lambda e: e.max(out=r8[:, 9, :], in_=selm), reads=[R_RT], writes=[R_r8])
    op("dve", lambda e: e.tensor_scalar(out=em, in0=selm, scalar1=r8[:, 9, 7:8], scalar2=None, op0=ALU.is_ge), reads=[R_RT, R_r8], writes=[R_RT])
    op("dve", lambda e: e.tensor_tensor(out=g_, in0=s_, in1=em, op=ALU.mult), reads=[R_RT], writes=[R_RT])
    op("dve", lambda e: e.tensor_reduce(out=r8[:, 9, 0:1], in_=g_, axis=mybir.AxisListType.X, op=ALU.add), reads=[R_RT, R_r8], writes=[R_r8])
    op("dve", lambda e: e.reciprocal(out=r8[:, 9, 1:2], in_=r8[:, 9, 0:1]), reads=[R_r8], writes=[R_r8])
    op("dve", lambda e: e.tensor_scalar(out=Gout, in0=g_, scalar1=r8[:, 9, 1:2], scalar2=2.5, op0=ALU.mult, op1=ALU.mult), reads=[R_RT, R_r8], writes=[R_Gout])


_PROG = {}


def _in_maps(inputs):
    c = make_consts()
    f = lambda a: np.ascontiguousarray(np.asarray(a, dtype=np.float32))
    bc = lambda v: np.ascontiguousarray(np.broadcast_to(np.asarray(v, np.float32)[None, :], (128, v.shape[-1])))
    shared = {
        "w_in": f(inputs["w_in"][0]), "w_a": f(inputs["w_branch_att"][0]), "w_h": f(inputs["w_branch_hgrn"][0]),
        "w_o": f(inputs["w_out"][0]), "w_r": f(inputs["router_w"][0]),
        "ewg": f(inputs["expert_w_gate"][0]), "ewu": f(inputs["expert_w_up"][0]), "ewd": f(inputs["expert_w_down"][0]),
        "swg": f(inputs["shared_w_gate"][0]), "swu": f(inputs["shared_w_up"][0]), "swd": f(inputs["shared_w_down"][0]),
        "w_pg": f(inputs["ple_gate_w"][0]), "w_pp": f(inputs["ple_proj_w"][0]),
        "v_lb0": bc(np.asarray(inputs["hgrn_lb_logits"])[0]), "v_lb1": bc(np.asarray(inputs["hgrn_lb_logits"])[1]),
        "v_gain": bc(np.asarray(inputs["hgrn_norm_w"])[0]),
        "v_ln1w": bc(np.asarray(inputs["ln1_w"])[0]), "v_ln1b": bc(np.asarray(inputs["ln1_b"])[0]),
        "v_ln2w": bc(np.asarray(inputs["ln2_w"])[0]), "v_ln2b": bc(np.asarray(inputs["ln2_b"])[0]),
        "v_rb": bc(np.asarray(inputs["router_bias"])[0]),
    }
    shared.update(c)
    x = np.asarray(inputs["x"], np.float32)
    p = np.asarray(inputs["p"], np.float32)[0]
    maps = []
    for b in range(x.shape[0]):
        m = dict(shared)
        m["x"] = np.ascontiguousarray(x[b])
        m["p"] = np.ascontiguousarray(p[b])
        maps.append(m)
    return maps


def kernel(**inputs):
    if "nc" not in _PROG:
        _PROG["nc"] = build_program("D")[0]
    nc = _PROG["nc"]
    maps = _in_maps(inputs)
    res = run_bass_kernel_spmd(nc, maps, core_ids=list(range(len(maps))))
    return np.stack([np.asarray(r["out"], dtype=np.float32) for r in res.results], axis=0)
```
